# Optimizing a Trainium2 kernel written in Bass

```python
import jax, jax.numpy as jnp
from jax import lax
import numpy as np

D_MODEL = 2048
BATCH = 2
SEQ = 16384
DEPTH = 1

D_MIX = D_MODEL
HEAD_DIM = 128
ATT_HEADS = (D_MIX // 2) // HEAD_DIM
D_ATT = ATT_HEADS * HEAD_DIM
D_RNN = D_MIX - D_ATT
RNN_BLOCKS = 8
RNN_BLOCK = D_RNN // RNN_BLOCKS
CONV_WIDTH = 4
LRU_C = 8.0
Q_BLOCK = 128
COL_Q = 0
COL_K = COL_Q + D_ATT
COL_V = COL_K + D_ATT
COL_F = COL_V + D_ATT
COL_RX = COL_F + ATT_HEADS
COL_RG = COL_RX + D_RNN
D_IN_PROJ = COL_RG + D_RNN
N_GROUPS = 4
EXPERTS_PER_GROUP = 8
N_EXPERTS = N_GROUPS * EXPERTS_PER_GROUP
TOP_K_INNER = 2
D_EXPERT = D_MODEL // 8
D_PLE = 256
FORGET_BIAS_INIT = 2.0
EPS = 1e-6

kernel_name = "hymba_fox_rglru_hmoe_layer"


def rms_norm(x, gain):
    x32 = x.astype(jnp.float32)
    y = x32 * lax.rsqrt(jnp.mean(x32 * x32, axis=-1, keepdims=True) + EPS)
    return (y * gain.astype(jnp.float32)).astype(x.dtype)


def forgetting_attention(q, k, v, log_f):
    b, s, h, dh = q.shape
    nb = s // Q_BLOCK
    scale = dh ** -0.5
    c = jnp.cumsum(log_f, axis=1).transpose(0, 2, 1)
    kh = k.transpose(0, 2, 1, 3)
    vh = v.transpose(0, 2, 1, 3)
    q_blocks = q.transpose(0, 2, 1, 3).reshape(b, h, nb, Q_BLOCK, dh).transpose(2, 0, 1, 3, 4)
    c_blocks = c.reshape(b, h, nb, Q_BLOCK).transpose(2, 0, 1, 3)
    k_pos = jnp.arange(s, dtype=jnp.int32)
    starts = jnp.arange(nb, dtype=jnp.int32) * Q_BLOCK

    def one_block(args):
        qb, cb, start = args
        logits = jnp.einsum('bhqd,bhkd->bhqk', qb, kh,
                            preferred_element_type=jnp.float32) * scale
        logits = logits + cb[..., :, None] - c[:, :, None, :]
        q_pos = start + jnp.arange(Q_BLOCK, dtype=jnp.int32)
        causal = k_pos[None, :] <= q_pos[:, None]
        logits = jnp.where(causal, logits, -jnp.inf)
        probs = jax.nn.softmax(logits, axis=-1)
        return jnp.einsum('bhqk,bhkd->bhqd', probs.astype(vh.dtype), vh)

    out = lax.map(one_block, (q_blocks, c_blocks, starts))
    return out.transpose(1, 0, 3, 2, 4).reshape(b, s, h * dh)


def causal_depthwise_conv(x, w, bias):
    y = lax.conv_general_dilated(
        x, w[:, None, :].astype(x.dtype), window_strides=(1,),
        padding=[(CONV_WIDTH - 1, 0)],
        dimension_numbers=('NWC', 'WIO', 'NWC'),
        feature_group_count=x.shape[-1])
    return y + bias


def rg_lru(x, w_a, b_a, w_i, b_i, lam):
    b, s, d = x.shape
    xb = x.reshape(b, s, RNN_BLOCKS, RNN_BLOCK)
    r = jax.nn.sigmoid(jnp.einsum('bsni,nij->bsnj', xb, w_a).reshape(b, s, d) + b_a)
    i = jax.nn.sigmoid(jnp.einsum('bsni,nij->bsnj', xb, w_i).reshape(b, s, d) + b_i)
    log_a = -LRU_C * r.astype(jnp.float32) * jax.nn.softplus(-lam.astype(jnp.float32))
    a = jnp.exp(log_a)
    u = jnp.sqrt(-jnp.expm1(2.0 * log_a)) * (i * x).astype(jnp.float32)

    def combine(left, right):
        a1, b1 = left
        a2, b2 = right
        return a1 * a2, a2 * b1 + b2

    _, h = lax.associative_scan(combine, (a, u), axis=1)
    return h.astype(x.dtype)


def hierarchical_moe(hn, w_router_group, w_router_expert, w_gate, w_up, w_down):
    b, s, d = hn.shape
    t = hn.reshape(b * s, d)
    g_probs = jax.nn.softmax(jnp.dot(t, w_router_group, preferred_element_type=jnp.float32), axis=-1)
    g_w, g_idx = lax.top_k(g_probs, 1)
    e_logits = jnp.dot(t, w_router_expert, preferred_element_type=jnp.float32)
    e_logits = e_logits.reshape(-1, N_GROUPS, EXPERTS_PER_GROUP)
    e_in_group = jnp.take_along_axis(e_logits, g_idx[:, :, None], axis=1)[:, 0]
    top_l, top_i = lax.top_k(e_in_group, TOP_K_INNER)
    weights = jax.nn.softmax(top_l, axis=-1) * g_w
    ids = g_idx * EXPERTS_PER_GROUP + top_i
    comb = jnp.sum(jax.nn.one_hot(ids, N_EXPERTS, dtype=jnp.float32) * weights[..., None], axis=1)
    out = jnp.zeros((b * s, d), jnp.float32)
    for e in range(N_EXPERTS):
        hid = jax.nn.silu(t @ w_gate[e]) * (t @ w_up[e])
        out = out + (hid @ w_down[e]).astype(jnp.float32) * comb[:, e:e + 1]
    return out.astype(hn.dtype).reshape(b, s, d)


def setup_inputs(seed: int = 0) -> dict:
    key = jax.random.key(seed)
    ks = jax.random.split(key, 32)
    f32 = jnp.float32

    def nrm(k, shape, scale):
        return jax.random.normal(k, shape, f32) * scale

    def gain(k, shape):
        return 1.0 + 0.02 * jax.random.normal(k, shape, f32)

    u = jax.random.uniform(ks[12], (DEPTH, D_RNN), f32, 0.9, 0.999)
    s_lam = u ** (1.0 / LRU_C)
    lru_lambda = jnp.log(s_lam) - jnp.log1p(-s_lam)
    return {
        "x": nrm(ks[0], (BATCH, SEQ, D_MODEL), 1.0),
        "p": nrm(ks[1], (DEPTH, BATCH, SEQ, D_PLE), 1.0),
        "mix_norm": gain(ks[2], (DEPTH, D_MODEL)),
        "w_in": nrm(ks[3], (DEPTH, D_MODEL, D_IN_PROJ), D_MODEL ** -0.5),
        "b_forget": FORGET_BIAS_INIT + 0.1 * jax.random.normal(ks[4], (DEPTH, ATT_HEADS), f32),
        "q_norm": gain(ks[5], (DEPTH, HEAD_DIM)),
        "k_norm": gain(ks[6], (DEPTH, HEAD_DIM)),
        "conv_w": nrm(ks[7], (DEPTH, CONV_WIDTH, D_RNN), CONV_WIDTH ** -0.5),
        "conv_b": nrm(ks[8], (DEPTH, D_RNN), 0.02),
        "w_rec_gate": nrm(ks[9], (DEPTH, RNN_BLOCKS, RNN_BLOCK, RNN_BLOCK), RNN_BLOCK ** -0.5),
        "b_rec_gate": nrm(ks[10], (DEPTH, D_RNN), 0.02),
        "w_in_gate": nrm(ks[11], (DEPTH, RNN_BLOCKS, RNN_BLOCK, RNN_BLOCK), RNN_BLOCK ** -0.5),
        "b_in_gate": nrm(ks[13], (DEPTH, D_RNN), 0.02),
        "lru_lambda": lru_lambda,
        "w_out": nrm(ks[14], (DEPTH, D_MIX, D_MODEL), D_MIX ** -0.5),
        "ffn_norm": gain(ks[15], (DEPTH, D_MODEL)),
        "w_router_group": nrm(ks[16], (DEPTH, D_MODEL, N_GROUPS), D_MODEL ** -0.5),
        "w_router_expert": nrm(ks[17], (DEPTH, D_MODEL, N_EXPERTS), D_MODEL ** -0.5),
        "w_expert_gate": nrm(ks[18], (DEPTH, N_EXPERTS, D_MODEL, D_EXPERT), D_MODEL ** -0.5),
        "w_expert_up": nrm(ks[19], (DEPTH, N_EXPERTS, D_MODEL, D_EXPERT), D_MODEL ** -0.5),
        "w_expert_down": nrm(ks[20], (DEPTH, N_EXPERTS, D_EXPERT, D_MODEL), D_EXPERT ** -0.5),
        "ple_norm": gain(ks[21], (DEPTH, D_MODEL)),
        "w_ple_gate": nrm(ks[22], (DEPTH, D_MODEL, D_MODEL), D_MODEL ** -0.5),
        "w_ple_up": nrm(ks[23], (DEPTH, D_PLE, D_MODEL), D_PLE ** -0.5),
    }


def reference(x, p, mix_norm, w_in, b_forget, q_norm, k_norm, conv_w, conv_b,
              w_rec_gate, b_rec_gate, w_in_gate, b_in_gate, lru_lambda, w_out,
              ffn_norm, w_router_group, w_router_expert, w_expert_gate,
              w_expert_up, w_expert_down, ple_norm, w_ple_gate, w_ple_up):
    h = x
    b, s, _ = x.shape
    for i in range(DEPTH):
        hn = rms_norm(h, mix_norm[i])
        proj = hn @ w_in[i]
        q = rms_norm(proj[..., COL_Q:COL_K].reshape(b, s, ATT_HEADS, HEAD_DIM), q_norm[i])
        k = rms_norm(proj[..., COL_K:COL_V].reshape(b, s, ATT_HEADS, HEAD_DIM), k_norm[i])
        v = proj[..., COL_V:COL_F].reshape(b, s, ATT_HEADS, HEAD_DIM)
        log_f = jax.nn.log_sigmoid((proj[..., COL_F:COL_RX] + b_forget[i]).astype(jnp.float32))
        att_out = forgetting_attention(q, k, v, log_f)

        rx = causal_depthwise_conv(proj[..., COL_RX:COL_RG], conv_w[i], conv_b[i])
        rnn = rg_lru(rx, w_rec_gate[i], b_rec_gate[i], w_in_gate[i], b_in_gate[i], lru_lambda[i])
        rnn_out = jax.nn.gelu(proj[..., COL_RG:D_IN_PROJ], approximate=True) * rnn

        h = h + jnp.concatenate([att_out, rnn_out], axis=-1) @ w_out[i]

        hn = rms_norm(h, ffn_norm[i])
        h = h + hierarchical_moe(hn, w_router_group[i], w_router_expert[i],
                                 w_expert_gate[i], w_expert_up[i], w_expert_down[i])

        gate = jax.nn.sigmoid(rms_norm(h, ple_norm[i]) @ w_ple_gate[i])
        h = h + gate * (p[i] @ w_ple_up[i])
    return h
```

```python
import os
import numpy as np
import ml_dtypes
from contextlib import ExitStack
import concourse.bass as bass
import concourse.mybir as mybir
from concourse.bass_utils import run_bass_kernel_spmd

F32 = mybir.dt.float32
BF16 = mybir.dt.bfloat16
AF = mybir.ActivationFunctionType
ALU = mybir.AluOpType
AX = mybir.AxisListType

ENGS = ('pe', 'act', 'dve', 'pool', 'sp')
EPS = 1e-6
S = 16384
D = 2048
NOWN = 4096
COL_K, COL_V, COL_F, COL_RX, COL_RG = 1024, 2048, 3072, 3080, 4104
DEBUG = False


class T:
    __slots__ = ('name', 'w', 'r')

    def __init__(self, name):
        self.name = name
        self.w = None
        self.r = {}


class Op:
    __slots__ = ('eng', 'fn', 'deps', 'sig', 'sigval', 'dkey', 'dval', 'idx')


class Prog:
    def __init__(self, nc):
        self.nc = nc
        self.ops = {e: [] for e in ENGS}
        self.dcnt = {}
        self.dlast = {}
        self.pending = {}

    def op(self, eng, fn, reads=(), writes=(), dkey=None):
        o = Op()
        o.eng, o.fn, o.sig, o.sigval, o.dkey, o.dval = eng, fn, False, 0, dkey, 0
        deps = []
        for t in reads:
            if t.w is not None:
                deps.append((t.w, 'raw'))
        for t in writes:
            if t.w is not None:
                deps.append((t.w, 'waw'))
            for d in t.r.values():
                deps.append((d, 'war'))
        if eng in self.pending:
            lasts, dl = self.pending.pop(eng)
            deps = deps + [(d, 'bar') for d in lasts if d.eng != eng] + [(d, 'bar') for d in dl]
        o.deps = deps
        rk = ('d', dkey) if dkey is not None else eng
        for t in reads:
            t.r[rk] = o
        for t in writes:
            t.w = o
            t.r = {}
        if dkey is not None:
            self.dcnt[dkey] = self.dcnt.get(dkey, 0) + 16
            o.dval = self.dcnt[dkey]
            self.dlast[dkey] = o
        o.idx = len(self.ops[eng])
        self.ops[eng].append(o)
        return o

    def barrier(self):
        lasts = [self.ops[e][-1] for e in ENGS if self.ops[e] and self.ops[e][-1].dkey is None]
        dl = list(self.dlast.values())
        self.pending = {e: (lasts, dl) for e in ENGS}

    @staticmethod
    def _need(o, d, kind):
        if d is o:
            return False
        if d.dkey is not None:
            return True
        if d.eng == o.eng and o.dkey is None:
            return kind == 'raw' and o.eng != 'pe'
        return True

    def emit(self, stack):
        nc = self.nc
        for e in ENGS:
            for o in self.ops[e]:
                for d, kind in o.deps:
                    if d.dkey is None and self._need(o, d, kind):
                        d.sig = True
        for e in ENGS:
            c = 0
            for o in self.ops[e]:
                if o.dkey is None and o.sig:
                    c += 1
                    o.sigval = c
        esem = {e: stack.enter_context(nc.semaphore("es_" + e)) for e in ENGS}
        dsem = {k: stack.enter_context(nc.semaphore("ds_" + str(k))) for k in self.dcnt}
        block = stack.enter_context(nc.Block())

        def run(e, h):
            waited = {}
            for o in self.ops[e]:
                need = {}
                for d, kind in o.deps:
                    if not self._need(o, d, kind):
                        continue
                    if d.dkey is not None:
                        k, v, s = ('d', d.dkey), d.dval, dsem[d.dkey]
                    else:
                        k, v, s = ('e', d.eng), d.sigval, esem[d.eng]
                    if waited.get(k, 0) >= v:
                        continue
                    if k not in need or need[k][0] < v:
                        need[k] = (v, s)
                for k, (v, s) in need.items():
                    h.wait_ge(s, v)
                    waited[k] = v
                inst = o.fn(h)
                if o.dkey is not None:
                    inst.then_inc(dsem[o.dkey], 16)
                elif o.sig:
                    inst.then_inc(esem[e], 1)

        @block.tensor
        def _(h):
            run('pe', h)

        @block.scalar
        def _(h):
            run('act', h)

        @block.vector
        def _(h):
            run('dve', h)

        @block.gpsimd
        def _(h):
            run('pool', h)

        @block.sync
        def _(h):
            run('sp', h)


class Arena:
    def __init__(self, tens, nbytes):
        self.t = tens
        self.n = nbytes
        self.off = 0

    def alloc(self, shape, dt):
        esz = 4 if dt == F32 else 2
        n = int(np.prod(shape[1:])) * esz
        n = (n + 63) // 64 * 64
        al = 64
        while al < n and al < 32768:
            al *= 2
        self.off = (self.off + al - 1) // al * al
        assert self.off + n <= self.n, ("arena overflow", self.off, n, self.n)
        v = self.t[:, self.off // 2:(self.off + n) // 2]
        self.off += n
        if dt == F32:
            v = v.bitcast(F32)
        cnt = int(np.prod(shape[1:]))
        v = v[:, 0:cnt]
        if len(shape) == 3:
            v = v.rearrange("p (a b) -> p a b", a=shape[1])
        elif len(shape) == 4:
            v = v.rearrange("p (a b c) -> p a b c", a=shape[1], b=shape[2])
        return v


def build(stop=9):
    nc = bass.Bass("TRN2", target_bir_lowering=False)
    P = Prog(nc)
    st = ExitStack()

    def din(name, shape, dt=F32):
        return nc.dram_tensor(name, shape, dt, kind="ExternalInput")

    def dscr(name, shape, dt=BF16):
        return nc.dram_tensor(name, shape, dt, kind=("ExternalOutput" if DEBUG else "Internal"))

    x_all = din("x_all", [S, D])
    x_own = din("x_own", [NOWN, D])
    p_own = din("p_own", [NOWN, 256])
    mix_norm = din("mix_norm", [1, D])
    w_in = din("w_in", [D, 5128])
    b_forget = din("b_forget", [1, 8])
    q_norm = din("q_norm", [128, 1])
    k_norm = din("k_norm", [128, 1])
    conv_w = din("conv_w", [4, 1024])
    conv_b = din("conv_b", [1, 1024])
    w_rec = din("w_rec_gate", [8, 128, 128])
    b_rec = din("b_rec_gate", [1, 1024])
    w_ing = din("w_in_gate", [8, 128, 128])
    b_ing = din("b_in_gate", [1, 1024])
    lam = din("lru_lambda", [1, 1024])
    w_out = din("w_out", [D, D])
    ffn_norm = din("ffn_norm", [1, D])
    w_rg = din("w_router_group", [D, 4])
    w_re = din("w_router_expert", [D, 32])
    w_eg = din("w_expert_gate", [32, D, 256])
    w_eu = din("w_expert_up", [32, D, 256])
    w_ed = din("w_expert_down", [32, 256, D])
    ple_norm = din("ple_norm", [1, D])
    w_pg = din("w_ple_gate", [D, D])
    w_pu = din("w_ple_up", [256, D])
    c_sel = din("c_sel", [128, 4])
    c_sel16 = din("c_sel16", [128, 128])
    c_mask = din("c_mask", [128, 16, 512], BF16)
    c_ident = din("c_ident", [128, 128], BF16)
    c_tri = din("c_tri", [128, 128])
    out_own = nc.dram_tensor("out_own", [NOWN, D], F32, kind="ExternalOutput")

    XT_s = dscr("XT_s", [S // 512, 128, 16, 512])
    XTO_s = dscr("XTO_s", [NOWN // 512, 128, 16, 512])
    KT_s = dscr("KT_s", [8, 128, S])
    V_s = dscr("V_s", [8, 128, S // 128, 128])
    HOWN_s = dscr("HOWN_s", [8, 128, NOWN])
    QT_s = dscr("QT_s", [8, 128, NOWN])
    CAT_s = dscr("CAT_s", [4, 128, 16, 1024])
    XD_s = dscr("XD_s", [2, 2, 128, 16, 512]) if DEBUG else None

    def sb(name, shape, dt):
        return st.enter_context(nc.sbuf_tensor(name, shape, dt))

    def bcast_row(handle, n, off=0):
        return bass.AP(handle, off, [[0, 128], [1, n]])

    cnt = [0]

    def newT(name="t"):
        cnt[0] += 1
        return T(name + str(cnt[0]))

    ARENA_BYTES = 176 * 1024
    arena_t = st.enter_context(nc.sbuf_tensor("arena", [128, ARENA_BYTES // 2], BF16, align_bytes=4096))
    AR = Arena(arena_t, ARENA_BYTES)
    ident = sb("ident", [128, 128], BF16)
    ones_bf = sb("ones_bf", [128, 128], BF16)
    tri = sb("tri", [128, 128], F32)
    ones_f = sb("ones_f", [128, 128], F32)
    sel = sb("sel", [128, 4], F32)
    sel16 = sb("sel16", [128, 128], F32)
    kgain = sb("kgain", [128, 1], F32)
    qgain = sb("qgain", [128, 1], F32)
    convb = sb("convb", [128, 8], F32)
    convw = sb("convw", [128, 4, 8], F32)
    ba = sb("ba", [128, 8], F32)
    bi = sb("bi", [128, 8], F32)
    cch = sb("cch", [128, 8], F32)
    cch2 = sb("cch2", [128, 8], F32)
    bf_rep = sb("bf_rep", [128, 8], F32)
    lf = sb("lf", [128, 8, 128], F32)
    ccol = sb("ccol", [128, 8, 128], F32)
    crefp = sb("crefp", [128, 8, 8], F32)
    hstate = sb("hstate", [128, 8], F32)
    small = sb("small", [128, 64], F32)
    T_const = T("const")
    T_small = T("small")

    pbanks = [st.enter_context(nc.psum_tensor("pb%d" % i, [128, 512], F32)) for i in range(8)]
    T_pb = [T("pb%d" % i) for i in range(8)]

    def ld(eng, out_ap, in_ap, key, writes, nc_ok=False):
        if nc_ok:
            P.op(eng, lambda h: h.dma_start(out=out_ap, in_=in_ap, allow_slow_non_contiguous=True), writes=writes, dkey=key)
        else:
            P.op(eng, lambda h: h.dma_start(out=out_ap, in_=in_ap), writes=writes, dkey=key)

    ld('sp', ident[:], c_ident.ap(), "c0", [T_const])
    ld('sp', tri[:], c_tri.ap(), "c1", [T_const])
    ld('sp', sel[:], c_sel.ap(), "c2", [T_const])
    ld('sp', sel16[:], c_sel16.ap(), "c3", [T_const])
    ld('sp', kgain[:], k_norm.ap(), "c4", [T_const])
    ld('sp', qgain[:], q_norm.ap(), "c5", [T_const])
    ld('sp', convb[:], conv_b.ap().rearrange("o (b p) -> p (o b)", p=128), "c6", [T_const], True)
    for k in range(4):
        ld('sp', convw[:, k, :], conv_w.ap()[k:k + 1, :].rearrange("o (b p) -> p (o b)", p=128), "c7", [T_const], True)
    ld('sp', ba[:], b_rec.ap().rearrange("o (b p) -> p (o b)", p=128), "c8", [T_const], True)
    ld('sp', bi[:], b_ing.ap().rearrange("o (b p) -> p (o b)", p=128), "c9", [T_const], True)
    ld('sp', cch[:], lam.ap().rearrange("o (b p) -> p (o b)", p=128), "c10", [T_const], True)
    ld('sp', bf_rep[:], bcast_row(b_forget, 8), "c11", [T_const])
    P.op('dve', lambda h: h.memset(ones_bf[:], 1.0), writes=[T_const])
    P.op('dve', lambda h: h.memset(ones_f[:], 1.0), writes=[T_const])
    P.op('dve', lambda h: h.memset(hstate[:], 0.0), writes=[T_const])
    P.op('act', lambda h: h.activation(out=cch[:], in_=cch[:], func=AF.Exp, scale=-1.0), reads=[T_const], writes=[T_const])
    P.op('act', lambda h: h.activation(out=cch[:], in_=cch[:], func=AF.Ln, bias=1.0), reads=[T_const], writes=[T_const])
    P.op('dve', lambda h: h.tensor_scalar(out=cch2[:], in0=cch[:], scalar1=-16.0, scalar2=None, op0=ALU.mult), reads=[T_const], writes=[T_const])
    P.op('dve', lambda h: h.tensor_scalar(out=cch[:], in0=cch[:], scalar1=-8.0, scalar2=None, op0=ALU.mult), reads=[T_const], writes=[T_const])
    P.op('dve', lambda h: h.tensor_scalar(out=qgain[:], in0=qgain[:], scalar1=float(128 ** -0.5), scalar2=None, op0=ALU.mult), reads=[T_const], writes=[T_const])

    def finish():
        P.barrier()
        P.op('sp', lambda h: h.nop())
        P.emit(st)
        st.close()
        return nc

    if stop == 0:
        return finish()
    def mm_group(bank_ap, pairs, bankT, reads, first=True, last=True):
        def fn(h):
            inst = None
            n = len(pairs)
            for i, (l, r) in enumerate(pairs):
                inst = h.matmul(bank_ap, lhsT=l, rhs=r, start=(first and i == 0), stop=(last and i == n - 1))
            return inst
        P.op('pe', fn, reads=reads, writes=[bankT])

    def transposes(bank_bf_ap, srcs, bankT, reads):
        def fn(h):
            inst = None
            for i, s_ap in enumerate(srcs):
                inst = h.transpose(bank_bf_ap[:, i * 128:(i + 1) * 128], s_ap, ident[:])
            return inst
        P.op('pe', fn, reads=reads + [T_const], writes=[bankT])

    pb_rr = [0]

    def next_bank(lo=0, hi=8):
        i = lo + pb_rr[0] % (hi - lo)
        pb_rr[0] += 1
        return i

    mark0 = AR.off
    gain_rep = AR.alloc([128, D], F32)
    T_gain = newT("gain")
    ld('sp', gain_rep, bcast_row(mix_norm, D), "g0", [T_gain])
    xf = [AR.alloc([128, D], F32) for _ in range(2)]
    T_xf = [newT("xf") for _ in range(2)]
    junk = AR.alloc([128, D], BF16)
    T_junk = newT("junk")
    xn = [AR.alloc([128, D], BF16) for _ in range(2)]
    T_xn = [newT("xn") for _ in range(2)]
    xTt = [AR.alloc([128, 16, 512], BF16) for _ in range(2)]
    T_xTt = [newT("xTt") for _ in range(2)]
    sm = [AR.alloc([128, 8], F32) for _ in range(2)]
    T_sm = [newT("sm") for _ in range(2)]

    def norm_tile(src_ap, slot, dst_bf, T_dst, grep, T_grep, key):
        ld('sp', xf[slot], src_ap, key + str(slot), [T_xf[slot]])
        s_ = sm[slot]
        P.op('act', lambda h: h.activation(out=junk, in_=xf[slot], func=AF.Square, accum_out=s_[:, 0:1]),
             reads=[T_xf[slot]], writes=[T_junk, T_sm[slot]])
        P.op('dve', lambda h: h.tensor_scalar(out=s_[:, 1:2], in0=s_[:, 0:1], scalar1=1.0 / D, scalar2=EPS, op0=ALU.mult, op1=ALU.add),
             reads=[T_sm[slot]], writes=[T_sm[slot]])
        P.op('act', lambda h: h.activation(out=s_[:, 2:3], in_=s_[:, 1:2], func=AF.Sqrt), reads=[T_sm[slot]], writes=[T_sm[slot]])
        P.op('dve', lambda h: h.reciprocal(out=s_[:, 3:4], in_=s_[:, 2:3]), reads=[T_sm[slot]], writes=[T_sm[slot]])
        P.op('dve', lambda h: h.scalar_tensor_tensor(out=dst_bf, in0=xf[slot], scalar=s_[:, 3:4], in1=grep, op0=ALU.mult, op1=ALU.mult),
             reads=[T_xf[slot], T_sm[slot], T_grep], writes=[T_dst])

    def p0_pass(src, dst, ntiles, key):
        def s1(i):
            slot = i % 2
            norm_tile(src.ap()[i * 128:(i + 1) * 128, :], slot, xn[slot], T_xn[slot], gain_rep, T_gain, key)

        def s2(i):
            slot = i % 2
            sts = (i // 4) % 2
            q = i % 4
            b0 = next_bank(0, 8)
            b1 = next_bank(0, 8)
            for half, b in enumerate((b0, b1)):
                transposes(pbanks[b][:].bitcast(BF16), [xn[slot][:, (half * 8 + kk) * 128:(half * 8 + kk + 1) * 128] for kk in range(8)],
                           T_pb[b], [T_xn[slot]])
                dstv = xTt[sts][:, half * 8:(half + 1) * 8, q * 128:(q + 1) * 128]
                srcv = pbanks[b][:].bitcast(BF16).rearrange("p (a b) -> p a b", a=8)
                if half == 0:
                    P.op('act', (lambda dstv, srcv: lambda h: h.activation(out=dstv, in_=srcv, func=AF.Copy))(dstv, srcv),
                         reads=[T_pb[b]], writes=[T_xTt[sts]])
                else:
                    P.op('dve', (lambda dstv, srcv: lambda h: h.tensor_copy(out=dstv, in_=srcv))(dstv, srcv),
                         reads=[T_pb[b]], writes=[T_xTt[sts]])
            if q == 3:
                stn = i // 4
                dv = dst.ap()[stn]
                P.op('sp', (lambda dv, sts: lambda h: h.dma_start(out=dv, in_=xTt[sts]))(dv, sts), reads=[T_xTt[sts]], dkey=key + "st" + str(sts))

        s1(0)
        for i in range(ntiles):
            if i + 1 < ntiles:
                s1(i + 1)
            s2(i)

    p0_pass(x_all, XT_s, S // 128, "p0a")
    p0_pass(x_own, XTO_s, NOWN // 128, "p0b")
    P.barrier()
    if stop == 1:
        return finish()

    AR.off = mark0
    Wk = AR.alloc([128, 16, 1024], BF16)
    Wv = AR.alloc([128, 16, 1024], BF16)
    Wf = AR.alloc([128, 16, 8], BF16)
    T_W = newT("W")
    T_Wp = []

    def load_w(dst, col0, ncols, key):
        for kq in range(4):
            srcv = w_in.ap()[kq * 512:(kq + 1) * 512, col0:col0 + ncols].rearrange("(k p) n -> p k n", p=128)
            tp = newT("Wp")
            T_Wp.append(tp)
            P.op('pool', (lambda dstv, srcv: lambda h: h.dma_start(out=dstv, in_=srcv))(dst[:, kq * 4:(kq + 1) * 4, :], srcv),
                 writes=[tp], dkey=key + str(kq))

    load_w(Wk, COL_K, 1024, "wk")
    load_w(Wv, COL_V, 1024, "wv")
    load_w(Wf, COL_F, 8, "wf")
    xT = [AR.alloc([128, 16, 512], BF16) for _ in range(2)]
    T_xT = [newT("xT") for _ in range(2)]
    ksq = [AR.alloc([128, 512], BF16) for _ in range(2)]
    T_ksq = [newT("ksq") for _ in range(2)]
    kstd = [AR.alloc([128, 512], F32) for _ in range(2)]
    T_kstd = [newT("kstd") for _ in range(2)]
    ktb = [AR.alloc([128, 512], BF16) for _ in range(2)]
    T_ktb = [newT("ktb") for _ in range(2)]
    vbig = [AR.alloc([128, 8, 4, 128], BF16) for _ in range(2)]
    T_vbig = [newT("vbig") for _ in range(2)]
    zf = [AR.alloc([128, 8], F32) for _ in range(2)]
    T_zf = [newT("zf") for _ in range(2)]
    T_lf = T("lf")
    T_hst = T("hstate")

    def qk_norm_unit(W, hcol, xTs, T_xTs, gain_ap, dst_dram, key, u):
        b = next_bank(0, 6)
        mm_group(pbanks[b][:], [(W[:, k, hcol * 128:(hcol + 1) * 128], xTs[:, k, :]) for k in range(16)], T_pb[b], [T_W, T_xTs] + T_Wp)
        s = u % 2
        ksq_s, kstd_s, ktb_s = ksq[s], kstd[s], ktb[s]
        Tq, Td, Tb = T_ksq[s], T_kstd[s], T_ktb[s]
        P.op('act', lambda h: h.activation(out=ksq_s, in_=pbanks[b][:], func=AF.Square), reads=[T_pb[b]], writes=[Tq])
        b2 = 6 + u % 2
        mm_group(pbanks[b2][:], [(ones_bf[:], ksq_s)], T_pb[b2], [T_const, Tq])
        P.op('act', lambda h: h.activation(out=kstd_s, in_=pbanks[b2][:], func=AF.Sqrt, scale=1.0 / 128, bias=EPS),
             reads=[T_pb[b2]], writes=[Td])
        P.op('dve', lambda h: h.reciprocal(out=kstd_s, in_=kstd_s), reads=[Td], writes=[Td])
        P.op('dve', lambda h: h.scalar_tensor_tensor(out=ktb_s, in0=pbanks[b][:], scalar=gain_ap, in1=kstd_s, op0=ALU.mult, op1=ALU.mult),
             reads=[T_pb[b], Td, T_const], writes=[Tb])
        P.op('sp', lambda h: h.dma_start(out=dst_dram, in_=ktb_s), reads=[Tb], dkey=key + str(s))

    ucnt = [0]
    for stn in range(S // 512):
        xs = stn % 2
        xTs, T_xTs = xT[xs], T_xT[xs]
        if stn == 0:
            ld('sp', xT[0], XT_s.ap()[0], "xTl0", [T_xT[0]])
        if stn + 1 < S // 512:
            ld('sp', xT[(stn + 1) % 2], XT_s.ap()[stn + 1], "xTl" + str((stn + 1) % 2), [T_xT[(stn + 1) % 2]])
        t0 = stn * 512
        if DEBUG and stn in (1, 3):
            P.op('sp', (lambda xTs, i: lambda h: h.dma_start(out=XD_s.ap()[i, 0], in_=xTs))(xTs, stn // 2),
                 reads=[T_xTs], dkey="xd0")
        for hh in range(8):
            qk_norm_unit(Wk, hh, xTs, T_xTs, kgain[:, 0:1], KT_s.ap()[hh, :, t0:t0 + 512], "kst", ucnt[0])
            ucnt[0] += 1
        for q in range(4):
            for half in range(2):
                b = next_bank(0, 6)
                mm_group(pbanks[b][:], [(xTs[:, k, q * 128:(q + 1) * 128], Wv[:, k, half * 512:(half + 1) * 512]) for k in range(16)],
                         T_pb[b], [T_W, T_xTs] + T_Wp)
                dstv = vbig[xs][:, half * 4:(half + 1) * 4, q, :]
                if half == 0:
                    P.op('act', (lambda dstv, b: lambda h: h.activation(out=dstv, in_=pbanks[b][:].rearrange("p (h d) -> p h d", h=4), func=AF.Copy))(dstv, b),
                         reads=[T_pb[b]], writes=[T_vbig[xs]])
                else:
                    P.op('dve', (lambda dstv, b: lambda h: h.tensor_copy(out=dstv, in_=pbanks[b][:].rearrange("p (h d) -> p h d", h=4)))(dstv, b),
                         reads=[T_pb[b]], writes=[T_vbig[xs]])
            b = next_bank(0, 6)
            mm_group(pbanks[b][:, 0:8], [(xTs[:, k, q * 128:(q + 1) * 128], Wf[:, k, :]) for k in range(16)], T_pb[b], [T_W, T_xTs] + T_Wp)
            s = q % 2
            tile_i = stn * 4 + q
            P.op('dve', (lambda s, b: lambda h: h.tensor_tensor(out=zf[s], in0=pbanks[b][:, 0:8], in1=bf_rep[:], op=ALU.add))(s, b),
                 reads=[T_pb[b], T_const], writes=[T_zf[s]])
            P.op('act', (lambda s: lambda h: h.activation(out=zf[s], in_=zf[s], func=AF.Exp, scale=-1.0))(s), reads=[T_zf[s]], writes=[T_zf[s]])
            P.op('act', (lambda s, tile_i: lambda h: h.activation(out=lf[:, :, tile_i], in_=zf[s], func=AF.Ln, bias=1.0))(s, tile_i),
                 reads=[T_zf[s]], writes=[T_lf])
        dvv = V_s.ap()[:, :, stn * 4:(stn + 1) * 4, :].rearrange("h p q d -> p h q d")
        P.op('sp', (lambda xs, dvv: lambda h: h.dma_start(out=dvv, in_=vbig[xs]))(xs, dvv), reads=[T_vbig[xs]], dkey="vst" + str(xs))
        if DEBUG and stn in (1, 3):
            P.op('sp', (lambda xTs, i: lambda h: h.dma_start(out=XD_s.ap()[i, 1], in_=xTs))(xTs, stn // 2),
                 reads=[T_xTs], dkey="xd1")
    P.barrier()
    if stop == 2:
        return finish()
    AR.off = mark0
    Wrx = AR.alloc([128, 16, 1024], BF16)
    Wa = AR.alloc([128, 8, 128], BF16)
    Wi = AR.alloc([128, 8, 128], BF16)
    dg = AR.alloc([128, 32, 128], BF16)
    T_W = newT("WB")
    T_Wp = []
    load_w(Wrx, COL_RX, 1024, "wrx")
    P.op('pool', lambda h: h.dma_start(out=Wa, in_=w_rec.ap().rearrange("b i j -> i b j")), writes=[T_W], dkey="wa")
    P.op('pool', lambda h: h.dma_start(out=Wi, in_=w_ing.ap().rearrange("b i j -> i b j")), writes=[T_W], dkey="wi")
    for blk in range(8):
        for k in range(4):
            P.op('dve', (lambda blk, k: lambda h: h.tensor_scalar(out=dg[:, blk * 4 + k, :], in0=ident[:], scalar1=convw[:, k, blk:blk + 1],
                                                                 scalar2=None, op0=ALU.mult))(blk, k), reads=[T_const], writes=[T_W])
    xT = [AR.alloc([128, 16, 512], BF16) for _ in range(2)]
    T_xT = [newT("xTB") for _ in range(2)]
    rxh = AR.alloc([128, 8, 516], BF16)
    T_rxh = [newT("rxh") for _ in range(8)]
    rxc = [AR.alloc([128, 512], BF16) for _ in range(2)]
    T_rxc = [newT("rxc") for _ in range(2)]
    rr = [AR.alloc([128, 512], F32) for _ in range(2)]
    T_rr = [newT("rr") for _ in range(2)]
    ig = [AR.alloc([128, 512], F32) for _ in range(2)]
    T_ig = [newT("ig") for _ in range(2)]
    aa = [AR.alloc([128, 512], F32) for _ in range(2)]
    T_aa = [newT("aa") for _ in range(2)]
    sq = [AR.alloc([128, 512], F32) for _ in range(2)]
    T_sq = [newT("sq") for _ in range(2)]
    uu = [AR.alloc([128, 512], F32) for _ in range(2)]
    T_uu = [newT("uu") for _ in range(2)]
    hs = [AR.alloc([128, 512], F32) for _ in range(2)]
    T_hs = [newT("hs") for _ in range(2)]
    hacc = AR.alloc([128, 8, 512], F32)
    T_hacc = [newT("hacc") for _ in range(8)]
    hob = [AR.alloc([128, 512], BF16) for _ in range(2)]
    T_hob = [newT("hob") for _ in range(2)]
    P.op('pool', lambda h: h.memset(rxh, 0.0), writes=T_rxh)
    def rnn_R1(stn, blk):
        xTs, T_xTs = xT[stn % 2], T_xT[stn % 2]
        s = blk % 2
        b = next_bank(0, 6)
        mm_group(pbanks[b][:], [(Wrx[:, k, blk * 128:(blk + 1) * 128], xTs[:, k, :]) for k in range(16)], T_pb[b], [T_W, T_xTs] + T_Wp)
        P.op('act', (lambda blk, b: lambda h: h.activation(out=rxh[:, blk, 3:515], in_=pbanks[b][:], func=AF.Copy))(blk, b),
             reads=[T_pb[b]], writes=[T_rxh[blk]])
        return b

    def rnn_R2(stn, blk):
        s = blk % 2
        b2 = next_bank(0, 6)
        mm_group(pbanks[b2][:], [(dg[:, blk * 4 + k, :], rxh[:, blk, k:k + 512]) for k in range(4)], T_pb[b2], [T_W, T_rxh[blk]])
        P.op('pool', (lambda blk: lambda h: h.tensor_copy(out=rxh[:, blk, 0:3], in_=rxh[:, blk, 512:515]))(blk),
             reads=[T_rxh[blk]], writes=[T_rxh[blk]])
        P.op('act', (lambda s, blk, b2: lambda h: h.activation(out=rxc[s], in_=pbanks[b2][:], func=AF.Identity, bias=convb[:, blk:blk + 1]))(s, blk, b2),
             reads=[T_pb[b2], T_const], writes=[T_rxc[s]])
        b3 = next_bank(0, 6)
        b4 = next_bank(0, 6)
        mm_group(pbanks[b3][:], [(Wa[:, blk, :], rxc[s])], T_pb[b3], [T_W, T_rxc[s]])
        mm_group(pbanks[b4][:], [(Wi[:, blk, :], rxc[s])], T_pb[b4], [T_W, T_rxc[s]])
        P.op('act', (lambda s, blk, b3: lambda h: h.activation(out=rr[s], in_=pbanks[b3][:], func=AF.Sigmoid, bias=ba[:, blk:blk + 1]))(s, blk, b3),
             reads=[T_pb[b3], T_const], writes=[T_rr[s]])
        P.op('act', (lambda s, blk, b4: lambda h: h.activation(out=ig[s], in_=pbanks[b4][:], func=AF.Sigmoid, bias=bi[:, blk:blk + 1]))(s, blk, b4),
             reads=[T_pb[b4], T_const], writes=[T_ig[s]])
        P.op('act', (lambda s, blk: lambda h: h.activation(out=aa[s], in_=rr[s], func=AF.Exp, scale=cch[:, blk:blk + 1]))(s, blk),
             reads=[T_rr[s], T_const], writes=[T_aa[s]])
        P.op('act', (lambda s, blk: lambda h: h.activation(out=sq[s], in_=rr[s], func=AF.Exp, scale=cch2[:, blk:blk + 1]))(s, blk),
             reads=[T_rr[s], T_const], writes=[T_sq[s]])
        P.op('act', (lambda s: lambda h: h.activation(out=sq[s], in_=sq[s], func=AF.Ln, scale=-1.0, bias=1.0))(s),
             reads=[T_sq[s]], writes=[T_sq[s]])
        P.op('act', (lambda s: lambda h: h.activation(out=sq[s], in_=sq[s], func=AF.Exp, scale=0.5))(s),
             reads=[T_sq[s]], writes=[T_sq[s]])
        P.op('pool', (lambda s: lambda h: h.tensor_tensor(out=uu[s], in0=ig[s], in1=rxc[s], op=ALU.mult))(s),
             reads=[T_ig[s], T_rxc[s]], writes=[T_uu[s]])
        P.op('dve', (lambda s: lambda h: h.tensor_tensor(out=uu[s], in0=uu[s], in1=sq[s], op=ALU.mult))(s),
             reads=[T_uu[s], T_sq[s]], writes=[T_uu[s]])
        P.op('dve', (lambda s, blk: lambda h: h.tensor_tensor_scan(out=hs[s], data0=aa[s], data1=uu[s], initial=hstate[:, blk:blk + 1],
                                                                  op0=ALU.mult, op1=ALU.add))(s, blk),
             reads=[T_aa[s], T_uu[s], T_hst], writes=[T_hs[s]])
        P.op('dve', (lambda s, blk: lambda h: h.tensor_copy(out=hstate[:, blk:blk + 1], in_=hs[s][:, 511:512]))(s, blk),
             reads=[T_hs[s]], writes=[T_hst])
        r4 = stn % 4
        if r4 == 0:
            P.op('dve', (lambda s, blk: lambda h: h.tensor_scalar(out=hacc[:, blk, :], in0=hs[s], scalar1=sel[:, 0:1], scalar2=None, op0=ALU.mult))(s, blk),
                 reads=[T_hs[s], T_const], writes=[T_hacc[blk]])
        elif r4 < 3:
            P.op('dve', (lambda s, blk, r4: lambda h: h.scalar_tensor_tensor(out=hacc[:, blk, :], in0=hs[s], scalar=sel[:, r4:r4 + 1], in1=hacc[:, blk, :],
                                                                             op0=ALU.mult, op1=ALU.add))(s, blk, r4),
                 reads=[T_hs[s], T_const, T_hacc[blk]], writes=[T_hacc[blk]])
        else:
            m = stn // 4
            P.op('dve', (lambda s, blk: lambda h: h.scalar_tensor_tensor(out=hob[s], in0=hs[s], scalar=sel[:, 3:4], in1=hacc[:, blk, :],
                                                                         op0=ALU.mult, op1=ALU.add))(s, blk),
                 reads=[T_hs[s], T_const, T_hacc[blk]], writes=[T_hob[s]])
            dv = HOWN_s.ap()[blk, :, m * 512:(m + 1) * 512]
            P.op('sp', (lambda s, dv: lambda h: h.dma_start(out=dv, in_=hob[s]))(s, dv), reads=[T_hob[s]], dkey="hst" + str(s))

    NJ = (S // 512) * 8
    ld('sp', xT[0], XT_s.ap()[0], "xTm0", [T_xT[0]])
    rnn_R1(0, 0)
    for jj in range(NJ):
        if jj % 8 == 0 and jj // 8 + 1 < S // 512:
            sn = jj // 8 + 1
            ld('sp', xT[sn % 2], XT_s.ap()[sn], "xTm" + str(sn % 2), [T_xT[sn % 2]])
        if jj + 1 < NJ:
            rnn_R1((jj + 1) // 8, (jj + 1) % 8)
        rnn_R2(jj // 8, jj % 8)
    P.barrier()
    if stop == 3:
        return finish()

    AR.off = mark0
    Wq = AR.alloc([128, 16, 1024], BF16)
    Wg = AR.alloc([128, 16, 1024], BF16)
    T_W = newT("W4")
    T_Wp = []
    load_w(Wq, 0, 1024, "wq")
    load_w(Wg, COL_RG, 1024, "wg")
    xT = [AR.alloc([128, 16, 512], BF16) for _ in range(2)]
    T_xT = [newT("xT4") for _ in range(2)]
    ksq = [AR.alloc([128, 512], BF16) for _ in range(2)]
    T_ksq = [newT("ksq4") for _ in range(2)]
    kstd = [AR.alloc([128, 512], F32) for _ in range(2)]
    T_kstd = [newT("kstd4") for _ in range(2)]
    ktb = [AR.alloc([128, 512], BF16) for _ in range(2)]
    T_ktb = [newT("ktb4") for _ in range(2)]
    gg = [AR.alloc([128, 512], F32) for _ in range(2)]
    T_gg = [newT("gg") for _ in range(2)]
    hw = [AR.alloc([128, 512], BF16) for _ in range(2)]
    T_hw = [newT("hw") for _ in range(2)]
    ro = [AR.alloc([128, 512], BF16) for _ in range(2)]
    T_ro = [newT("ro") for _ in range(2)]
    for m in range(8):
        xs = m % 2
        xTs, T_xTs = xT[xs], T_xT[xs]
        if m == 0:
            ld('sp', xT[0], XTO_s.ap()[0], "xTo0", [T_xT[0]])
        if m + 1 < 8:
            ld('sp', xT[(m + 1) % 2], XTO_s.ap()[m + 1], "xTo" + str((m + 1) % 2), [T_xT[(m + 1) % 2]])
        for hh in range(8):
            qk_norm_unit(Wq, hh, xTs, T_xTs, qgain[:, 0:1], QT_s.ap()[hh, :, m * 512:(m + 1) * 512], "qst", ucnt[0])
            ucnt[0] += 1
        for blk in range(8):
            s = blk % 2
            b = next_bank(0, 6)
            mm_group(pbanks[b][:], [(Wg[:, k, blk * 128:(blk + 1) * 128], xTs[:, k, :]) for k in range(16)], T_pb[b], [T_W, T_xTs] + T_Wp)
            ld('sp', hw[s], HOWN_s.ap()[blk, :, m * 512:(m + 1) * 512], "hwl" + str(s), [T_hw[s]])
            P.op('act', (lambda s, b: lambda h: h.activation(out=gg[s], in_=pbanks[b][:], func=AF.Gelu_apprx_tanh))(s, b),
                 reads=[T_pb[b]], writes=[T_gg[s]])
            P.op('dve', (lambda s: lambda h: h.tensor_tensor(out=ro[s], in0=gg[s], in1=hw[s], op=ALU.mult))(s),
                 reads=[T_gg[s], T_hw[s]], writes=[T_ro[s]])
            dv = CAT_s.ap()[m // 2, :, 8 + blk, (m % 2) * 512:(m % 2 + 1) * 512]
            P.op('sp', (lambda s, dv: lambda h: h.dma_start(out=dv, in_=ro[s]))(s, dv), reads=[T_ro[s]], dkey="rost" + str(s))

    oi = AR.alloc([128, 128], F32)
    T_oi = newT("oi")
    tmpc = AR.alloc([128, 128], F32)
    T_tmpc = newT("tmpc")
    for hh in range(8):
        bw = next_bank(0, 6)
        bt_ = next_bank(0, 6)
        mm_group(pbanks[bw][:, 0:128], [(tri[:], lf[:, hh, :])], T_pb[bw], [T_const, T_lf])
        mm_group(pbanks[bt_][:, 0:128], [(ones_f[:], lf[:, hh, :])], T_pb[bt_], [T_const, T_lf])
        P.op('dve', (lambda bt_: lambda h: h.tensor_tensor_scan(out=oi, data0=ones_f[:], data1=pbanks[bt_][:, 0:128], initial=0.0,
                                                                op0=ALU.mult, op1=ALU.add))(bt_),
             reads=[T_pb[bt_], T_const], writes=[T_oi])
        P.op('dve', (lambda bw, hh: lambda h: h.tensor_tensor(out=ccol[:, hh, :], in0=pbanks[bw][:, 0:128], in1=oi, op=ALU.add))(bw, hh),
             reads=[T_pb[bw], T_oi], writes=[T_small])
        P.op('dve', (lambda bt_, hh: lambda h: h.tensor_tensor(out=ccol[:, hh, :], in0=ccol[:, hh, :], in1=pbanks[bt_][:, 0:128], op=ALU.subtract))(bt_, hh),
             reads=[T_pb[bt_], T_small], writes=[T_small])
        P.op('dve', lambda h: h.tensor_tensor(out=tmpc, in0=oi, in1=sel16[:], op=ALU.mult), reads=[T_oi, T_const], writes=[T_tmpc])
        P.op('dve', (lambda hh: lambda h: h.tensor_reduce(out=crefp[:, hh, :], in_=tmpc.rearrange("p (m r) -> p m r", r=16), axis=AX.X, op=ALU.add))(hh),
             reads=[T_tmpc], writes=[T_small])
    P.barrier()
    if stop == 4:
        return finish()

    AR.off = mark0
    KT = [AR.alloc([128, S], BF16) for _ in range(2)]
    VV = [AR.alloc([128, 128, 128], BF16) for _ in range(2)]
    maskb = AR.alloc([128, 16, 512], BF16)
    T_mask = newT("mask")
    ld('sp', maskb, c_mask.ap(), "mask", [T_mask])
    QT = [AR.alloc([128, NOWN], BF16) for _ in range(2)]
    T_kvq = [newT("kvq") for _ in range(2)]
    pex = [AR.alloc([128, 512], BF16) for _ in range(4)]
    T_pex = [newT("pex") for _ in range(4)]
    biasb = [AR.alloc([128, 128], F32) for _ in range(2)]
    T_bias = [newT("bias") for _ in range(2)]
    rden = [AR.alloc([128, 512], F32) for _ in range(2)]
    T_rden = [newT("rden") for _ in range(2)]
    ob = [AR.alloc([128, 512], BF16) for _ in range(2)]
    T_ob = [newT("ob") for _ in range(2)]
    pacc = [AR.alloc([128, 512], F32) for _ in range(2)]
    T_pacc = [newT("pacc") for _ in range(2)]
    sidx = [0]
    for hh in range(8):
        hsl = hh % 2
        key = "kvq" + str(hsl)
        for c4 in range(4):
            ld('sp', KT[hsl][:, c4 * 4096:(c4 + 1) * 4096], KT_s.ap()[hh, :, c4 * 4096:(c4 + 1) * 4096], key, [T_kvq[hsl]])
        for c4 in range(4):
            ld('sp', VV[hsl][:, c4 * 32:(c4 + 1) * 32, :], V_s.ap()[hh, :, c4 * 32:(c4 + 1) * 32, :], key, [T_kvq[hsl]])
        ld('sp', QT[hsl], QT_s.ap()[hh, :, :], key, [T_kvq[hsl]])
        for m in range(8):
            nkb = 16 * m + 16
            ms = m % 2
            bO, bD = 4 + 2 * ms, 5 + 2 * ms
            P.op('dve', (lambda ms, hh, m, nkb: lambda h: h.tensor_scalar(out=biasb[ms][:, 0:nkb], in0=ccol[:, hh, 0:nkb], scalar1=crefp[:, hh, m:m + 1],
                                                                          scalar2=None, op0=ALU.subtract))(ms, hh, m, nkb),
                 reads=[T_small], writes=[T_bias[ms]])
            pend = []
            for step in range(nkb + 2):
                if step < nkb:
                    kb = step
                    si = sidx[0] % 4
                    sidx[0] += 1
                    pairs = [(KT[hsl][:, kb * 128:(kb + 1) * 128], QT[hsl][:, m * 512:(m + 1) * 512])]
                    rds = [T_kvq[hsl]]
                    if kb >= 16 * m:
                        pairs.append((ident[:], maskb[:, kb - 16 * m, :]))
                        rds = rds + [T_mask, T_const]
                    mm_group(pbanks[si][:], pairs, T_pb[si], rds)
                    P.op('act', (lambda si, ms, kb: lambda h: h.activation(out=pex[si], in_=pbanks[si][:], func=AF.Exp, bias=biasb[ms][:, kb:kb + 1]))(si, ms, kb),
                         reads=[T_pb[si], T_bias[ms]], writes=[T_pex[si]])
                    pend.append((kb, si))
                if step >= 2:
                    kb, si = pend.pop(0)
                    mm_group(pbanks[bO][:], [(VV[hsl][:, kb, :], pex[si])], T_pb[bO], [T_kvq[hsl], T_pex[si]], first=(kb == 0), last=(kb == nkb - 1))
                    pa_ = kb % 2
                    if kb < 2:
                        P.op('dve', (lambda pa_, si: lambda h: h.tensor_copy(out=pacc[pa_], in_=pex[si]))(pa_, si), reads=[T_pex[si]], writes=[T_pacc[pa_]])
                    else:
                        P.op('dve', (lambda pa_, si: lambda h: h.tensor_tensor(out=pacc[pa_], in0=pacc[pa_], in1=pex[si], op=ALU.add))(pa_, si),
                             reads=[T_pex[si], T_pacc[pa_]], writes=[T_pacc[pa_]])
            mm_group(pbanks[bD][:], [(ones_f[:], pacc[0]), (ones_f[:], pacc[1])], T_pb[bD], [T_const, T_pacc[0], T_pacc[1]])
            P.op('dve', (lambda ms, bD: lambda h: h.reciprocal(out=rden[ms], in_=pbanks[bD][:]))(ms, bD), reads=[T_pb[bD]], writes=[T_rden[ms]])
            P.op('dve', (lambda ms, bO: lambda h: h.tensor_tensor(out=ob[ms], in0=pbanks[bO][:], in1=rden[ms], op=ALU.mult))(ms, bO),
                 reads=[T_pb[bO], T_rden[ms]], writes=[T_ob[ms]])
            dv = CAT_s.ap()[m // 2, :, hh, (m % 2) * 512:(m % 2 + 1) * 512]
            P.op('sp', (lambda ms, dv: lambda h: h.dma_start(out=dv, in_=ob[ms]))(ms, dv), reads=[T_ob[ms]], dkey="obst" + str(ms))
    P.barrier()
    if stop == 5:
        return finish()

    AR.off = mark0
    acc = AR.alloc([128, 8, D], F32)
    T_acc = [newT("acc") for _ in range(8)]
    hnT = AR.alloc([128, 16, 1024], BF16)
    T_hnT = newT("hnT")
    wch = [AR.alloc([128, 16, 512], BF16) for _ in range(2)]
    T_wch = [newT("wch") for _ in range(2)]
    wd = [AR.alloc([128, 2, D], BF16) for _ in range(1)]
    T_wd = [newT("wd") for _ in range(1)]
    grep7 = AR.alloc([128, D], F32)
    T_grep7 = newT("grep7")
    hn = [AR.alloc([128, D], BF16) for _ in range(1)]
    T_hn = [newT("hn") for _ in range(1)]
    wr = AR.alloc([128, 16, 36], BF16)
    T_wr = newT("wr")
    comb = AR.alloc([128, 8, 32], F32)
    T_comb = newT("comb")
    rt = AR.alloc([128, 160], F32)
    T_rt = newT("rt")
    sm7 = [AR.alloc([128, 8], F32) for _ in range(2)]
    T_sm7 = [newT("sm7") for _ in range(2)]
    sgb = [AR.alloc([128, 256], F32) for _ in range(2)]
    T_sgb = [newT("sgb") for _ in range(2)]
    hid = [AR.alloc([128, 256], BF16) for _ in range(2)]
    T_hid = [newT("hid") for _ in range(2)]
    hidT = [AR.alloc([128, 2, 128], BF16) for _ in range(2)]
    T_hidT = [newT("hidT") for _ in range(2)]
    ppf = AR.alloc([128, 256], F32)
    T_ppf = newT("ppf")
    ppb = AR.alloc([128, 256], BF16)
    T_ppb = newT("ppb")
    ppT = AR.alloc([128, 2, 1024], BF16)
    T_ppT = newT("ppT")
    wpu = [AR.alloc([128, 2, 512], BF16) for _ in range(1)]
    T_wpu = [newT("wpu") for _ in range(1)]
    sgm = [AR.alloc([128, 512], F32) for _ in range(1)]
    T_sgm = [newT("sgm") for _ in range(1)]
    catT = hnT
    wst = [AR.alloc([128, 1024], F32) for _ in range(2)]
    wst = wst + [grep7[:, 0:1024], grep7[:, 1024:2048]]
    T_wst = [newT("wst") for _ in range(4)]
    stc = [0]

    def stage_load(src_ap, a, bdim):
        i = stc[0] % 4
        stc[0] += 1
        view = wst[i][:, 0:a * bdim].rearrange("p (a b) -> p a b", a=a)
        ld('sp', view, src_ap, "wst" + str(i), [T_wst[i]])
        return i, view

    def stage_do_cast(eng, dst_ap, view, i, T_dst):
        P.op(eng, lambda h: h.tensor_copy(out=dst_ap, in_=view), reads=[T_wst[i]], writes=[T_dst])

    def stage_cast(dst_ap, src_ap, a, bdim, T_dst, eng='pool'):
        i, view = stage_load(src_ap, a, bdim)
        stage_do_cast(eng, dst_ap, view, i, T_dst)

    for kq in range(4):
        P.op('pool', (lambda kq: lambda h: h.dma_start(out=wr[:, kq * 4:(kq + 1) * 4, 0:4],
                                                       in_=w_rg.ap()[kq * 512:(kq + 1) * 512, :].rearrange("(k p) n -> p k n", p=128)))(kq),
             writes=[T_wr], dkey="wr")
        P.op('pool', (lambda kq: lambda h: h.dma_start(out=wr[:, kq * 4:(kq + 1) * 4, 4:36],
                                                       in_=w_re.ap()[kq * 512:(kq + 1) * 512, :].rearrange("(k p) n -> p k n", p=128)))(kq),
             writes=[T_wr], dkey="wr")

    wrr = [0]

    def load_wchunk(parts):
        s = wrr[0] % 2
        wrr[0] += 1
        for (c0, ncl, src) in parts:
            for kq in range(4):
                for cc in range(0, ncl, 256):
                    srcv = src[kq * 512:(kq + 1) * 512, cc:cc + 256].rearrange("(k p) n -> p k n", p=128)
                    stage_cast(wch[s][:, kq * 4:(kq + 1) * 4, c0 + cc:c0 + cc + 256], srcv, 4, 256, T_wch[s])
        return s

    def norm_block(gain_dram, key):
        ld('sp', grep7, bcast_row(gain_dram, D), key, [T_grep7, T_wst[2], T_wst[3]])
        for t in range(8):
            s = 0
            s_ = sm7[t % 2]
            P.op('act', (lambda t, s_: lambda h: h.activation(out=hn[0], in_=acc[:, t, :], func=AF.Square, accum_out=s_[:, 0:1]))(t, s_),
                 reads=[T_acc[t]], writes=[T_hn[0], T_sm7[t % 2]])
            P.op('dve', (lambda s_: lambda h: h.tensor_scalar(out=s_[:, 1:2], in0=s_[:, 0:1], scalar1=1.0 / D, scalar2=EPS, op0=ALU.mult, op1=ALU.add))(s_),
                 reads=[T_sm7[t % 2]], writes=[T_sm7[t % 2]])
            P.op('act', (lambda s_: lambda h: h.activation(out=s_[:, 2:3], in_=s_[:, 1:2], func=AF.Sqrt))(s_), reads=[T_sm7[t % 2]], writes=[T_sm7[t % 2]])
            P.op('dve', (lambda s_: lambda h: h.reciprocal(out=s_[:, 3:4], in_=s_[:, 2:3]))(s_), reads=[T_sm7[t % 2]], writes=[T_sm7[t % 2]])
            P.op('dve', (lambda t, s, s_: lambda h: h.scalar_tensor_tensor(out=hn[s], in0=acc[:, t, :], scalar=s_[:, 3:4], in1=grep7, op0=ALU.mult, op1=ALU.mult))(t, s, s_),
                 reads=[T_acc[t], T_sm7[t % 2], T_grep7, T_wst[2], T_wst[3]], writes=[T_hn[s]])
            for half in range(2):
                b = next_bank(0, 2)
                transposes(pbanks[b][:].bitcast(BF16), [hn[s][:, (half * 8 + kk) * 128:(half * 8 + kk + 1) * 128] for kk in range(8)], T_pb[b], [T_hn[s]])
                dstv = hnT[:, half * 8:(half + 1) * 8, t * 128:(t + 1) * 128]
                srcv = pbanks[b][:].bitcast(BF16).rearrange("p (a b) -> p a b", a=8)
                P.op('act', (lambda dstv, srcv: lambda h: h.activation(out=dstv, in_=srcv, func=AF.Copy))(dstv, srcv), reads=[T_pb[b]], writes=[T_hnT])


    for tb in range(4):
        o0 = tb * 1024
        ld('sp', catT, CAT_s.ap()[tb], "catl", [T_hnT])
        for t in range(8):
            ld('sp', acc[:, t, :], x_own.ap()[o0 + t * 128:o0 + (t + 1) * 128, :], "accl" + str(t), [T_acc[t]])
        for nch in range(4):
            s = load_wchunk([(0, 512, w_out.ap()[:, nch * 512:(nch + 1) * 512])])
            for t in range(8):
                b = next_bank(0, 2)
                mm_group(pbanks[b][:], [(catT[:, k, t * 128:(t + 1) * 128], wch[s][:, k, :]) for k in range(16)], T_pb[b], [T_hnT, T_wch[s]])
                P.op('dve', (lambda t, nch, b: lambda h: h.tensor_tensor(out=acc[:, t, nch * 512:(nch + 1) * 512], in0=acc[:, t, nch * 512:(nch + 1) * 512],
                                                                        in1=pbanks[b][:], op=ALU.add))(t, nch, b),
                     reads=[T_pb[b], T_acc[t]], writes=[T_acc[t]])
        norm_block(ffn_norm, "gr7")
        for t in range(8):
            b = next_bank(0, 2)
            mm_group(pbanks[b][:, 0:36], [(hnT[:, k, t * 128:(t + 1) * 128], wr[:, k, :]) for k in range(16)], T_pb[b], [T_hnT, T_wr])
            lg = rt[:, 0:36]
            gmax = rt[:, 36:37]
            gsh = rt[:, 40:44]
            gsum = rt[:, 44:45]
            gw = rt[:, 45:46]
            goh = rt[:, 48:52]
            em = rt[:, 56:88]
            m8 = rt[:, 88:96]
            d21 = rt[:, 96:97]
            w1 = rt[:, 97:98]
            w2 = rt[:, 98:99]
            c2 = rt[:, 104:136]
            ge = rt[:, 136:140]

            def R(eng, fn, extra_r=(), extra_w=()):
                P.op(eng, fn, reads=[T_rt] + list(extra_r), writes=[T_rt] + list(extra_w))

            R('dve', (lambda b: lambda h: h.tensor_copy(out=lg, in_=pbanks[b][:, 0:36]))(b), extra_r=[T_pb[b]])
            R('dve', lambda h: h.tensor_reduce(out=gmax, in_=lg[:, 0:4], axis=AX.X, op=ALU.max))
            R('dve', lambda h: h.tensor_scalar(out=gsh, in0=lg[:, 0:4], scalar1=gmax, scalar2=None, op0=ALU.subtract))
            R('act', lambda h: h.activation(out=ge, in_=gsh, func=AF.Exp, accum_out=gsum))
            R('dve', lambda h: h.reciprocal(out=gw, in_=gsum))
            R('dve', lambda h: h.tensor_scalar(out=goh, in0=gsh, scalar1=0.0, scalar2=None, op0=ALU.is_ge))
            R('dve', lambda h: h.tensor_scalar(out=goh, in0=goh, scalar1=-1.0, scalar2=1e9, op0=ALU.add, op1=ALU.mult))
            for g in range(4):
                R('dve', (lambda g: lambda h: h.tensor_scalar(out=em[:, g * 8:(g + 1) * 8], in0=lg[:, 4 + g * 8:4 + (g + 1) * 8], scalar1=goh[:, g:g + 1],
                                                               scalar2=None, op0=ALU.add))(g))
            R('dve', lambda h: h.max(out=m8, in_=em))
            R('dve', lambda h: h.tensor_tensor(out=d21, in0=m8[:, 1:2], in1=m8[:, 0:1], op=ALU.subtract))
            R('act', lambda h: h.activation(out=d21, in_=d21, func=AF.Exp))
            R('dve', lambda h: h.tensor_scalar(out=w1, in0=d21, scalar1=1.0, scalar2=None, op0=ALU.add))
            R('dve', lambda h: h.reciprocal(out=w1, in_=w1))
            R('dve', lambda h: h.tensor_tensor(out=w2, in0=d21, in1=w1, op=ALU.mult))
            R('dve', lambda h: h.tensor_tensor(out=w1, in0=w1, in1=gw, op=ALU.mult))
            R('dve', lambda h: h.tensor_tensor(out=w2, in0=w2, in1=gw, op=ALU.mult))
            R('dve', lambda h: h.tensor_scalar(out=c2, in0=em, scalar1=m8[:, 1:2], scalar2=w2, op0=ALU.is_equal, op1=ALU.mult))
            R('dve', (lambda t: lambda h: h.tensor_scalar(out=comb[:, t, :], in0=em, scalar1=m8[:, 0:1], scalar2=w1, op0=ALU.is_equal, op1=ALU.mult))(t),
              extra_w=[T_comb])
            R('dve', (lambda t: lambda h: h.tensor_tensor(out=comb[:, t, :], in0=comb[:, t, :], in1=c2, op=ALU.add))(t), extra_r=[T_comb], extra_w=[T_comb])
        def wch_pieces(e):
            s_ = e % 2
            out = []
            for (c0, src) in ((0, w_eg.ap()[e]), (256, w_eu.ap()[e])):
                for kq in range(4):
                    out.append((wch[s_][:, kq * 4:(kq + 1) * 4, c0:c0 + 256],
                                src[kq * 512:(kq + 1) * 512, :].rearrange("(k p) n -> p k n", p=128), T_wch[s_]))
            return out

        def wd_pieces(e):
            out = []
            for k2 in range(2):
                for cq in range(2):
                    out.append((wd[0][:, k2:k2 + 1, cq * 1024:(cq + 1) * 1024],
                                w_ed.ap()[e][k2 * 128:(k2 + 1) * 128, cq * 1024:(cq + 1) * 1024].rearrange("(k p) n -> p k n", p=128)))
            return out

        wd_pending = {}

        def stA(i):
            e, t = i // 8, i % 8
            u = i % 2
            b = i % 2
            s_ = e % 2
            mm_group(pbanks[b][:], [(hnT[:, k, t * 128:(t + 1) * 128], wch[s_][:, k, :]) for k in range(16)], T_pb[b], [T_hnT, T_wch[s_]])
            P.op('act', lambda h: h.activation(out=sgb[u], in_=pbanks[b][:, 0:256], func=AF.Silu), reads=[T_pb[b]], writes=[T_sgb[u]])
            P.op('dve', lambda h: h.scalar_tensor_tensor(out=hid[u], in0=pbanks[b][:, 256:512], scalar=comb[:, t, e:e + 1], in1=sgb[u],
                                                         op0=ALU.mult, op1=ALU.mult),
                 reads=[T_pb[b], T_sgb[u], T_comb], writes=[T_hid[u]])

        def stB(i):
            u = i % 2
            bT = 2 + u
            transposes(pbanks[bT][:].bitcast(BF16), [hid[u][:, 0:128], hid[u][:, 128:256]], T_pb[bT], [T_hid[u]])
            P.op('act', lambda h: h.activation(out=hidT[u], in_=pbanks[bT][:].bitcast(BF16)[:, 0:256].rearrange("p (a b) -> p a b", a=2), func=AF.Copy),
                 reads=[T_pb[bT]], writes=[T_hidT[u]])

        def stC(i):
            e, t = i // 8, i % 8
            u = i % 2
            if t == 0:
                if e not in wd_pending:
                    wd_pending[e] = [(dstp2, ) + stage_load(srcp2, 1, 1024) for (dstp2, srcp2) in wd_pieces(e)]
                for (dstp2, si, view) in wd_pending.pop(e):
                    stage_do_cast('dve', dstp2, view, si, T_wd[0])
            for nch in range(4):
                bd = 4 + nch
                mm_group(pbanks[bd][:], [(hidT[u][:, k, :], wd[0][:, k, nch * 512:(nch + 1) * 512]) for k in range(2)], T_pb[bd], [T_hidT[u], T_wd[0]])
                P.op('dve', (lambda nch, bd: lambda h: h.tensor_tensor(out=acc[:, t, nch * 512:(nch + 1) * 512], in0=acc[:, t, nch * 512:(nch + 1) * 512],
                                                                      in1=pbanks[bd][:], op=ALU.add))(nch, bd),
                     reads=[T_pb[bd], T_acc[t]], writes=[T_acc[t]])
            if e + 1 < 32:
                for pi in {0: (0, 1), 1: (2, 3), 2: (4,), 3: (5,), 4: (6,), 5: (7,)}.get(t, ()):
                    dstp, srcp, Tp = wch_pieces(e + 1)[pi]
                    stage_cast(dstp, srcp, 4, 256, Tp, eng='pool')
                if t == 7:
                    wd_pending[e + 1] = [(dstp2, ) + stage_load(srcp2, 1, 1024) for (dstp2, srcp2) in wd_pieces(e + 1)]

        for (dstp, srcp, Tp) in wch_pieces(0):
            stage_cast(dstp, srcp, 4, 256, Tp, eng='pool')
        NST = 256
        stA(0)
        stA(1)
        for i in range(NST):
            stB(i)
            if i + 2 < NST:
                stA(i + 2)
            stC(i)
        norm_block(ple_norm, "gr7")
        for t in range(8):
            ld('sp', ppf, p_own.ap()[o0 + t * 128:o0 + (t + 1) * 128, :], "ppl", [T_ppf])
            P.op('dve', lambda h: h.tensor_copy(out=ppb, in_=ppf), reads=[T_ppf], writes=[T_ppb])
            bT = 2 + t % 2
            transposes(pbanks[bT][:].bitcast(BF16), [ppb[:, 0:128], ppb[:, 128:256]], T_pb[bT], [T_ppb])
            P.op('act', (lambda t, bT: lambda h: h.activation(out=ppT[:, :, t * 128:(t + 1) * 128],
                                                              in_=pbanks[bT][:].bitcast(BF16)[:, 0:256].rearrange("p (a b) -> p a b", a=2), func=AF.Copy))(t, bT),
                 reads=[T_pb[bT]], writes=[T_ppT])
        for nch in range(4):
            s = load_wchunk([(0, 512, w_pg.ap()[:, nch * 512:(nch + 1) * 512])])
            ws = 0
            stage_cast(wpu[ws], w_pu.ap()[:, nch * 512:(nch + 1) * 512].rearrange("(k p) n -> p k n", p=128), 2, 512, T_wpu[ws])
            for t in range(8):
                u = 0
                b = next_bank(0, 2)
                mm_group(pbanks[b][:], [(hnT[:, k, t * 128:(t + 1) * 128], wch[s][:, k, :]) for k in range(16)], T_pb[b], [T_hnT, T_wch[s]])
                b2 = 4 + t % 2
                mm_group(pbanks[b2][:], [(ppT[:, k, t * 128:(t + 1) * 128], wpu[ws][:, k, :]) for k in range(2)], T_pb[b2], [T_ppT, T_wpu[ws]])
                P.op('act', (lambda u, b: lambda h: h.activation(out=sgm[u], in_=pbanks[b][:], func=AF.Sigmoid))(u, b), reads=[T_pb[b]], writes=[T_sgm[u]])
                P.op('dve', (lambda u, b2: lambda h: h.tensor_tensor(out=sgm[u], in0=sgm[u], in1=pbanks[b2][:], op=ALU.mult))(u, b2),
                     reads=[T_pb[b2], T_sgm[u]], writes=[T_sgm[u]])
                P.op('pool', (lambda t, nch, u: lambda h: h.tensor_tensor(out=acc[:, t, nch * 512:(nch + 1) * 512], in0=acc[:, t, nch * 512:(nch + 1) * 512],
                                                                         in1=sgm[u], op=ALU.add))(t, nch, u),
                     reads=[T_sgm[u], T_acc[t]], writes=[T_acc[t]])
        for t in range(8):
            P.op('sp', (lambda t, o0: lambda h: h.dma_start(out=out_own.ap()[o0 + t * 128:o0 + (t + 1) * 128, :], in_=acc[:, t, :]))(t, o0),
                 reads=[T_acc[t]], dkey="outst" + str(t))
    return finish()


_CACHE = {}


def _consts(j):
    selv = np.zeros((128, 4), np.float32)
    selv[:, j] = 1.0
    s16 = np.zeros((128, 8, 16), np.float32)
    s16[:, :, 4 * j + 1] = 1.0
    s16 = s16.reshape(128, 128)
    sl = np.arange(128)[:, None, None]
    r = np.arange(16)[None, :, None]
    t = np.arange(512)[None, None, :]
    mask = np.where(128 * r + sl - t - 512 * j > 0, -30000.0, 0.0).astype(ml_dtypes.bfloat16)
    ident = np.eye(128, dtype=np.float32).astype(ml_dtypes.bfloat16)
    tri = (np.arange(128)[:, None] <= np.arange(128)[None, :]).astype(np.float32)
    return dict(c_sel=selv, c_sel16=s16, c_mask=mask, c_ident=ident, c_tri=tri)


def kernel(**inputs):
    inp = {k: np.asarray(v) for k, v in inputs.items()}
    if "nc" not in _CACHE:
        _CACHE["nc"] = build(int(os.environ.get("K_STOP", "9")))
    nc = _CACHE["nc"]
    x = inp["x"]
    p = inp["p"][0]
    shared = {
        "mix_norm": inp["mix_norm"][0][None, :], "w_in": inp["w_in"][0], "b_forget": inp["b_forget"][0][None, :],
        "q_norm": inp["q_norm"][0][:, None], "k_norm": inp["k_norm"][0][:, None],
        "conv_w": inp["conv_w"][0], "conv_b": inp["conv_b"][0][None, :],
        "w_rec_gate": inp["w_rec_gate"][0], "b_rec_gate": inp["b_rec_gate"][0][None, :],
        "w_in_gate": inp["w_in_gate"][0], "b_in_gate": inp["b_in_gate"][0][None, :],
        "lru_lambda": inp["lru_lambda"][0][None, :], "w_out": inp["w_out"][0], "ffn_norm": inp["ffn_norm"][0][None, :],
        "w_router_group": inp["w_router_group"][0], "w_router_expert": inp["w_router_expert"][0],
        "w_expert_gate": inp["w_expert_gate"][0], "w_expert_up": inp["w_expert_up"][0], "w_expert_down": inp["w_expert_down"][0],
        "ple_norm": inp["ple_norm"][0][None, :], "w_ple_gate": inp["w_ple_gate"][0], "w_ple_up": inp["w_ple_up"][0],
    }
    shared = {k: np.ascontiguousarray(v, dtype=np.float32) for k, v in shared.items()}
    in_maps = []
    for c in range(8):
        b, j = c // 4, c % 4
        xo = np.ascontiguousarray(x[b].reshape(8, 4, 512, D)[:, j].reshape(NOWN, D))
        po = np.ascontiguousarray(p[b].reshape(8, 4, 512, 256)[:, j].reshape(NOWN, 256))
        m = dict(shared)
        m.update(x_all=np.ascontiguousarray(x[b]), x_own=xo, p_own=po)
        m.update(_consts(j))
        in_maps.append(m)
    res = run_bass_kernel_spmd(nc, in_maps, core_ids=list(range(8)))
    out = np.empty((2, S, D), np.float32)
    for c in range(8):
        b, j = c // 4, c % 4
        out[b].reshape(8, 4, 512, D)[:, j] = res.results[c]["out_own"].reshape(8, 512, D)
    if DEBUG:
        _CACHE["res"] = res
    return out
```

```python
import os
import numpy as np
import ml_dtypes
from contextlib import ExitStack
import concourse.bass as bass
import concourse.mybir as mybir
from concourse.bass_utils import run_bass_kernel_spmd

F32 = mybir.dt.float32
BF16 = mybir.dt.bfloat16
AF = mybir.ActivationFunctionType
ALU = mybir.AluOpType
AX = mybir.AxisListType

ENGS = ('pe', 'act', 'dve', 'pool', 'sp')
EPS = 1e-6
S = 16384
D = 2048
NOWN = 4096
COL_K, COL_V, COL_F, COL_RX, COL_RG = 1024, 2048, 3072, 3080, 4104
DEBUG = False


class T:
    __slots__ = ('name', 'w', 'r')

    def __init__(self, name):
        self.name = name
        self.w = None
        self.r = {}


class Op:
    __slots__ = ('eng', 'fn', 'deps', 'sig', 'sigval', 'dkey', 'dval', 'idx')


class Prog:
    def __init__(self, nc):
        self.nc = nc
        self.ops = {e: [] for e in ENGS}
        self.dcnt = {}
        self.dlast = {}
        self.pending = {}

    def op(self, eng, fn, reads=(), writes=(), dkey=None):
        o = Op()
        o.eng, o.fn, o.sig, o.sigval, o.dkey, o.dval = eng, fn, False, 0, dkey, 0
        deps = []
        for t in reads:
            if t.w is not None:
                deps.append((t.w, 'raw'))
        for t in writes:
            if t.w is not None:
                deps.append((t.w, 'waw'))
            for d in t.r.values():
                deps.append((d, 'war'))
        if eng in self.pending:
            lasts, dl = self.pending.pop(eng)
            deps = deps + [(d, 'bar') for d in lasts if d.eng != eng] + [(d, 'bar') for d in dl]
        o.deps = deps
        rk = ('d', dkey) if dkey is not None else eng
        for t in reads:
            t.r[rk] = o
        for t in writes:
            t.w = o
            t.r = {}
        if dkey is not None:
            self.dcnt[dkey] = self.dcnt.get(dkey, 0) + 16
            o.dval = self.dcnt[dkey]
            self.dlast[dkey] = o
        o.idx = len(self.ops[eng])
        self.ops[eng].append(o)
        return o

    def barrier(self):
        lasts = [self.ops[e][-1] for e in ENGS if self.ops[e] and self.ops[e][-1].dkey is None]
        dl = list(self.dlast.values())
        self.pending = {e: (lasts, dl) for e in ENGS}

    @staticmethod
    def _need(o, d, kind):
        if d is o:
            return False
        if d.dkey is not None:
            return True
        if d.eng == o.eng and o.dkey is None:
            return kind == 'raw' and o.eng != 'pe'
        return True

    def emit(self, stack):
        nc = self.nc
        for e in ENGS:
            for o in self.ops[e]:
                for d, kind in o.deps:
                    if d.dkey is None and self._need(o, d, kind):
                        d.sig = True
        for e in ENGS:
            c = 0
            for o in self.ops[e]:
                if o.dkey is None and o.sig:
                    c += 1
                    o.sigval = c
        esem = {e: stack.enter_context(nc.semaphore("es_" + e)) for e in ENGS}
        dsem = {k: stack.enter_context(nc.semaphore("ds_" + str(k))) for k in self.dcnt}
        block = stack.enter_context(nc.Block())

        def run(e, h):
            waited = {}
            for o in self.ops[e]:
                need = {}
                for d, kind in o.deps:
                    if not self._need(o, d, kind):
                        continue
                    if d.dkey is not None:
                        k, v, s = ('d', d.dkey), d.dval, dsem[d.dkey]
                    else:
                        k, v, s = ('e', d.eng), d.sigval, esem[d.eng]
                    if waited.get(k, 0) >= v:
                        continue
                    if k not in need or need[k][0] < v:
                        need[k] = (v, s)
                for k, (v, s) in need.items():
                    h.wait_ge(s, v)
                    waited[k] = v
                inst = o.fn(h)
                if o.dkey is not None:
                    inst.then_inc(dsem[o.dkey], 16)
                elif o.sig:
                    inst.then_inc(esem[e], 1)

        @block.tensor
        def _(h):
            run('pe', h)

        @block.scalar
        def _(h):
            run('act', h)

        @block.vector
        def _(h):
            run('dve', h)

        @block.gpsimd
        def _(h):
            run('pool', h)

        @block.sync
        def _(h):
            run('sp', h)


class Arena:
    def __init__(self, tens, nbytes):
        self.t = tens
        self.n = nbytes
        self.off = 0

    def alloc(self, shape, dt):
        esz = 4 if dt == F32 else 2
        n = int(np.prod(shape[1:])) * esz
        n = (n + 63) // 64 * 64
        al = 64
        while al < n and al < 32768:
            al *= 2
        self.off = (self.off + al - 1) // al * al
        assert self.off + n <= self.n, ("arena overflow", self.off, n, self.n)
        v = self.t[:, self.off // 2:(self.off + n) // 2]
        self.off += n
        if dt == F32:
            v = v.bitcast(F32)
        cnt = int(np.prod(shape[1:]))
        v = v[:, 0:cnt]
        if len(shape) == 3:
            v = v.rearrange("p (a b) -> p a b", a=shape[1])
        elif len(shape) == 4:
            v = v.rearrange("p (a b c) -> p a b c", a=shape[1], b=shape[2])
        return v


def build(stop=9):
    nc = bass.Bass("TRN2", target_bir_lowering=False)
    P = Prog(nc)
    st = ExitStack()

    def din(name, shape, dt=F32):
        return nc.dram_tensor(name, shape, dt, kind="ExternalInput")

    def dscr(name, shape, dt=BF16):
        return nc.dram_tensor(name, shape, dt, kind=("ExternalOutput" if DEBUG else "Internal"))

    x_all = din("x_all", [S, D])
    x_own = din("x_own", [NOWN, D])
    p_own = din("p_own", [NOWN, 256])
    mix_norm = din("mix_norm", [1, D])
    w_in = din("w_in", [D, 5128])
    b_forget = din("b_forget", [1, 8])
    q_norm = din("q_norm", [128, 1])
    k_norm = din("k_norm", [128, 1])
    conv_w = din("conv_w", [4, 1024])
    conv_b = din("conv_b", [1, 1024])
    w_rec = din("w_rec_gate", [8, 128, 128])
    b_rec = din("b_rec_gate", [1, 1024])
    w_ing = din("w_in_gate", [8, 128, 128])
    b_ing = din("b_in_gate", [1, 1024])
    lam = din("lru_lambda", [1, 1024])
    w_out = din("w_out", [D, D])
    ffn_norm = din("ffn_norm", [1, D])
    w_rg = din("w_router_group", [D, 4])
    w_re = din("w_router_expert", [D, 32])
    w_eg = din("w_expert_gate", [32, D, 256])
    w_eu = din("w_expert_up", [32, D, 256])
    w_ed = din("w_expert_down", [32, 256, D])
    ple_norm = din("ple_norm", [1, D])
    w_pg = din("w_ple_gate", [D, D])
    w_pu = din("w_ple_up", [256, D])
    c_sel = din("c_sel", [128, 4])
    c_sel16 = din("c_sel16", [128, 128])
    c_mask = din("c_mask", [128, 16, 512], BF16)
    c_ident = din("c_ident", [128, 128], BF16)
    c_tri = din("c_tri", [128, 128])
    out_own = nc.dram_tensor("out_own", [NOWN, D], F32, kind="ExternalOutput")

    XT_s = dscr("XT_s", [S // 512, 128, 16, 512])
    XTO_s = dscr("XTO_s", [NOWN // 512, 128, 16, 512])
    KT_s = dscr("KT_s", [8, 128, S])
    V_s = dscr("V_s", [8, 128, S // 128, 128])
    HOWN_s = dscr("HOWN_s", [8, 128, NOWN])
    QT_s = dscr("QT_s", [8, 128, NOWN])
    CAT_s = dscr("CAT_s", [4, 128, 16, 1024])
    XD_s = dscr("XD_s", [2, 2, 128, 16, 512]) if DEBUG else None

    def sb(name, shape, dt):
        return st.enter_context(nc.sbuf_tensor(name, shape, dt))

    def bcast_row(handle, n, off=0):
        return bass.AP(handle, off, [[0, 128], [1, n]])

    cnt = [0]

    def newT(name="t"):
        cnt[0] += 1
        return T(name + str(cnt[0]))

    ARENA_BYTES = 176 * 1024
    arena_t = st.enter_context(nc.sbuf_tensor("arena", [128, ARENA_BYTES // 2], BF16, align_bytes=4096))
    AR = Arena(arena_t, ARENA_BYTES)
    ident = sb("ident", [128, 128], BF16)
    ones_bf = sb("ones_bf", [128, 128], BF16)
    tri = sb("tri", [128, 128], F32)
    ones_f = sb("ones_f", [128, 128], F32)
    sel = sb("sel", [128, 4], F32)
    sel16 = sb("sel16", [128, 128], F32)
    kgain = sb("kgain", [128, 1], F32)
    qgain = sb("qgain", [128, 1], F32)
    convb = sb("convb", [128, 8], F32)
    convw = sb("convw", [128, 4, 8], F32)
    ba = sb("ba", [128, 8], F32)
    bi = sb("bi", [128, 8], F32)
    cch = sb("cch", [128, 8], F32)
    cch2 = sb("cch2", [128, 8], F32)
    bf_rep = sb("bf_rep", [128, 8], F32)
    lf = sb("lf", [128, 8, 128], F32)
    ccol = sb("ccol", [128, 8, 128], F32)
    crefp = sb("crefp", [128, 8, 8], F32)
    hstate = sb("hstate", [128, 8], F32)
    small = sb("small", [128, 64], F32)
    T_const = T("const")
    T_small = T("small")

    pbanks = [st.enter_context(nc.psum_tensor("pb%d" % i, [128, 512], F32)) for i in range(8)]
    T_pb = [T("pb%d" % i) for i in range(8)]

    def ld(eng, out_ap, in_ap, key, writes, nc_ok=False):
        if nc_ok:
            P.op(eng, lambda h: h.dma_start(out=out_ap, in_=in_ap, allow_slow_non_contiguous=True), writes=writes, dkey=key)
        else:
            P.op(eng, lambda h: h.dma_start(out=out_ap, in_=in_ap), writes=writes, dkey=key)

    ld('sp', ident[:], c_ident.ap(), "c0", [T_const])
    ld('sp', tri[:], c_tri.ap(), "c1", [T_const])
    ld('sp', sel[:], c_sel.ap(), "c2", [T_const])
    ld('sp', sel16[:], c_sel16.ap(), "c3", [T_const])
    ld('sp', kgain[:], k_norm.ap(), "c4", [T_const])
    ld('sp', qgain[:], q_norm.ap(), "c5", [T_const])
    ld('sp', convb[:], conv_b.ap().rearrange("o (b p) -> p (o b)", p=128), "c6", [T_const], True)
    for k in range(4):
        ld('sp', convw[:, k, :], conv_w.ap()[k:k + 1, :].rearrange("o (b p) -> p (o b)", p=128), "c7", [T_const], True)
    ld('sp', ba[:], b_rec.ap().rearrange("o (b p) -> p (o b)", p=128), "c8", [T_const], True)
    ld('sp', bi[:], b_ing.ap().rearrange("o (b p) -> p (o b)", p=128), "c9", [T_const], True)
    ld('sp', cch[:], lam.ap().rearrange("o (b p) -> p (o b)", p=128), "c10", [T_const], True)
    ld('sp', bf_rep[:], bcast_row(b_forget, 8), "c11", [T_const])
    P.op('dve', lambda h: h.memset(ones_bf[:], 1.0), writes=[T_const])
    P.op('dve', lambda h: h.memset(ones_f[:], 1.0), writes=[T_const])
    P.op('dve', lambda h: h.memset(hstate[:], 0.0), writes=[T_const])
    P.op('act', lambda h: h.activation(out=cch[:], in_=cch[:], func=AF.Exp, scale=-1.0), reads=[T_const], writes=[T_const])
    P.op('act', lambda h: h.activation(out=cch[:], in_=cch[:], func=AF.Ln, bias=1.0), reads=[T_const], writes=[T_const])
    P.op('dve', lambda h: h.tensor_scalar(out=cch2[:], in0=cch[:], scalar1=-16.0, scalar2=None, op0=ALU.mult), reads=[T_const], writes=[T_const])
    P.op('dve', lambda h: h.tensor_scalar(out=cch[:], in0=cch[:], scalar1=-8.0, scalar2=None, op0=ALU.mult), reads=[T_const], writes=[T_const])
    P.op('dve', lambda h: h.tensor_scalar(out=qgain[:], in0=qgain[:], scalar1=float(128 ** -0.5), scalar2=None, op0=ALU.mult), reads=[T_const], writes=[T_const])

    def finish():
        P.barrier()
        P.op('sp', lambda h: h.nop())
        P.emit(st)
        st.close()
        return nc

    if stop == 0:
        return finish()
    def mm_group(bank_ap, pairs, bankT, reads, first=True, last=True):
        def fn(h):
            inst = None
            n = len(pairs)
            for i, (l, r) in enumerate(pairs):
                inst = h.matmul(bank_ap, lhsT=l, rhs=r, start=(first and i == 0), stop=(last and i == n - 1))
            return inst
        P.op('pe', fn, reads=reads, writes=[bankT])

    def transposes(bank_bf_ap, srcs, bankT, reads):
        def fn(h):
            inst = None
            for i, s_ap in enumerate(srcs):
                inst = h.transpose(bank_bf_ap[:, i * 128:(i + 1) * 128], s_ap, ident[:])
            return inst
        P.op('pe', fn, reads=reads + [T_const], writes=[bankT])

    pb_rr = [0]

    def next_bank(lo=0, hi=8):
        i = lo + pb_rr[0] % (hi - lo)
        pb_rr[0] += 1
        return i

    mark0 = AR.off
    gain_rep = AR.alloc([128, D], F32)
    T_gain = newT("gain")
    ld('sp', gain_rep, bcast_row(mix_norm, D), "g0", [T_gain])
    xf = [AR.alloc([128, D], F32) for _ in range(4)]
    T_xf = [newT("xf") for _ in range(4)]
    junk = AR.alloc([128, D], BF16)
    T_junk = newT("junk")
    xn = [AR.alloc([128, D], BF16) for _ in range(4)]
    T_xn = [newT("xn") for _ in range(4)]
    xTt = [AR.alloc([128, 16, 512], BF16) for _ in range(2)]
    T_xTt = [newT("xTt") for _ in range(2)]
    sm = [AR.alloc([128, 8], F32) for _ in range(4)]
    T_sm = [newT("sm") for _ in range(4)]

    def norm_tile(src_ap, slot, dst_bf, T_dst, grep, T_grep, key):
        ld('sp', xf[slot], src_ap, key + str(slot), [T_xf[slot]])
        s_ = sm[slot]
        P.op('act', lambda h: h.activation(out=junk, in_=xf[slot], func=AF.Square, accum_out=s_[:, 0:1]),
             reads=[T_xf[slot]], writes=[T_junk, T_sm[slot]])
        P.op('dve', lambda h: h.tensor_scalar(out=s_[:, 1:2], in0=s_[:, 0:1], scalar1=1.0 / D, scalar2=EPS, op0=ALU.mult, op1=ALU.add),
             reads=[T_sm[slot]], writes=[T_sm[slot]])
        P.op('act', lambda h: h.activation(out=s_[:, 2:3], in_=s_[:, 1:2], func=AF.Sqrt), reads=[T_sm[slot]], writes=[T_sm[slot]])
        P.op('dve', lambda h: h.reciprocal(out=s_[:, 3:4], in_=s_[:, 2:3]), reads=[T_sm[slot]], writes=[T_sm[slot]])
        P.op('dve', lambda h: h.scalar_tensor_tensor(out=dst_bf, in0=xf[slot], scalar=s_[:, 3:4], in1=grep, op0=ALU.mult, op1=ALU.mult),
             reads=[T_xf[slot], T_sm[slot], T_grep], writes=[T_dst])

    def p0_pass(src, dst, ntiles, key):
        def s1(i):
            slot = i % 4
            norm_tile(src.ap()[i * 128:(i + 1) * 128, :], slot, xn[slot], T_xn[slot], gain_rep, T_gain, key)

        def s2(i):
            slot = i % 4
            sts = (i // 4) % 2
            q = i % 4
            b0 = next_bank(0, 8)
            b1 = next_bank(0, 8)
            for half, b in enumerate((b0, b1)):
                transposes(pbanks[b][:].bitcast(BF16), [xn[slot][:, (half * 8 + kk) * 128:(half * 8 + kk + 1) * 128] for kk in range(8)],
                           T_pb[b], [T_xn[slot]])
                dstv = xTt[sts][:, half * 8:(half + 1) * 8, q * 128:(q + 1) * 128]
                srcv = pbanks[b][:].bitcast(BF16).rearrange("p (a b) -> p a b", a=8)
                if half == 0:
                    P.op('act', (lambda dstv, srcv: lambda h: h.activation(out=dstv, in_=srcv, func=AF.Copy))(dstv, srcv),
                         reads=[T_pb[b]], writes=[T_xTt[sts]])
                else:
                    P.op('dve', (lambda dstv, srcv: lambda h: h.tensor_copy(out=dstv, in_=srcv))(dstv, srcv),
                         reads=[T_pb[b]], writes=[T_xTt[sts]])
            if q == 3:
                stn = i // 4
                dv = dst.ap()[stn]
                P.op('sp', (lambda dv, sts: lambda h: h.dma_start(out=dv, in_=xTt[sts]))(dv, sts), reads=[T_xTt[sts]], dkey=key + "st" + str(sts))

        s1(0)
        s1(1)
        for i in range(ntiles):
            if i + 2 < ntiles:
                s1(i + 2)
            s2(i)

    p0_pass(x_all, XT_s, S // 128, "p0a")
    p0_pass(x_own, XTO_s, NOWN // 128, "p0a")
    P.barrier()
    if stop == 1:
        return finish()

    AR.off = mark0
    Wk = AR.alloc([128, 16, 1024], BF16)
    Wv = AR.alloc([128, 16, 1024], BF16)
    Wf = AR.alloc([128, 16, 8], BF16)
    T_W = newT("W")
    T_Wp = []

    def load_w(dst, col0, ncols, key):
        for kq in range(4):
            srcv = w_in.ap()[kq * 512:(kq + 1) * 512, col0:col0 + ncols].rearrange("(k p) n -> p k n", p=128)
            tp = newT("Wp")
            T_Wp.append(tp)
            P.op('pool', (lambda dstv, srcv: lambda h: h.dma_start(out=dstv, in_=srcv))(dst[:, kq * 4:(kq + 1) * 4, :], srcv),
                 writes=[tp], dkey=key + str(kq))

    load_w(Wk, COL_K, 1024, "wk")
    load_w(Wv, COL_V, 1024, "wv")
    load_w(Wf, COL_F, 8, "wf")
    xT = [AR.alloc([128, 16, 512], BF16) for _ in range(2)]
    T_xT = [newT("xT") for _ in range(2)]
    ksq = [AR.alloc([128, 512], BF16) for _ in range(2)]
    T_ksq = [newT("ksq") for _ in range(2)]
    kstd = [AR.alloc([128, 512], F32) for _ in range(2)]
    T_kstd = [newT("kstd") for _ in range(2)]
    ktb = [AR.alloc([128, 512], BF16) for _ in range(2)]
    T_ktb = [newT("ktb") for _ in range(2)]
    vbig = [AR.alloc([128, 8, 4, 128], BF16) for _ in range(2)]
    T_vbig = [newT("vbig") for _ in range(2)]
    zf = [AR.alloc([128, 8], F32) for _ in range(2)]
    T_zf = [newT("zf") for _ in range(2)]
    T_lf = T("lf")
    T_hst = T("hstate")

    def qk_norm_unit(W, hcol, xTs, T_xTs, gain_ap, dst_dram, key, u):
        b = next_bank(0, 6)
        mm_group(pbanks[b][:], [(W[:, k, hcol * 128:(hcol + 1) * 128], xTs[:, k, :]) for k in range(16)], T_pb[b], [T_W, T_xTs] + T_Wp)
        s = u % 2
        ksq_s, kstd_s, ktb_s = ksq[s], kstd[s], ktb[s]
        Tq, Td, Tb = T_ksq[s], T_kstd[s], T_ktb[s]
        P.op('act', lambda h: h.activation(out=ksq_s, in_=pbanks[b][:], func=AF.Square), reads=[T_pb[b]], writes=[Tq])
        b2 = 6 + u % 2
        mm_group(pbanks[b2][:], [(ones_bf[:], ksq_s)], T_pb[b2], [T_const, Tq])
        P.op('act', lambda h: h.activation(out=kstd_s, in_=pbanks[b2][:], func=AF.Sqrt, scale=1.0 / 128, bias=EPS),
             reads=[T_pb[b2]], writes=[Td])
        P.op('dve', lambda h: h.reciprocal(out=kstd_s, in_=kstd_s), reads=[Td], writes=[Td])
        P.op('dve', lambda h: h.scalar_tensor_tensor(out=ktb_s, in0=pbanks[b][:], scalar=gain_ap, in1=kstd_s, op0=ALU.mult, op1=ALU.mult),
             reads=[T_pb[b], Td, T_const], writes=[Tb])
        P.op('sp', lambda h: h.dma_start(out=dst_dram, in_=ktb_s), reads=[Tb], dkey=key + str(s))

    ucnt = [0]
    for stn in range(S // 512):
        xs = stn % 2
        xTs, T_xTs = xT[xs], T_xT[xs]
        if stn == 0:
            ld('sp', xT[0], XT_s.ap()[0], "xTl0", [T_xT[0]])
        if stn + 1 < S // 512:
            ld('sp', xT[(stn + 1) % 2], XT_s.ap()[stn + 1], "xTl" + str((stn + 1) % 2), [T_xT[(stn + 1) % 2]])
        t0 = stn * 512
        if DEBUG and stn in (1, 3):
            P.op('sp', (lambda xTs, i: lambda h: h.dma_start(out=XD_s.ap()[i, 0], in_=xTs))(xTs, stn // 2),
                 reads=[T_xTs], dkey="xd0")
        for hh in range(8):
            qk_norm_unit(Wk, hh, xTs, T_xTs, kgain[:, 0:1], KT_s.ap()[hh, :, t0:t0 + 512], "kst", ucnt[0])
            ucnt[0] += 1
        for q in range(4):
            for half in range(2):
                b = next_bank(0, 6)
                mm_group(pbanks[b][:], [(xTs[:, k, q * 128:(q + 1) * 128], Wv[:, k, half * 512:(half + 1) * 512]) for k in range(16)],
                         T_pb[b], [T_W, T_xTs] + T_Wp)
                dstv = vbig[xs][:, half * 4:(half + 1) * 4, q, :]
                if half == 0:
                    P.op('act', (lambda dstv, b: lambda h: h.activation(out=dstv, in_=pbanks[b][:].rearrange("p (h d) -> p h d", h=4), func=AF.Copy))(dstv, b),
                         reads=[T_pb[b]], writes=[T_vbig[xs]])
                else:
                    P.op('dve', (lambda dstv, b: lambda h: h.tensor_copy(out=dstv, in_=pbanks[b][:].rearrange("p (h d) -> p h d", h=4)))(dstv, b),
                         reads=[T_pb[b]], writes=[T_vbig[xs]])
            b = next_bank(0, 6)
            mm_group(pbanks[b][:, 0:8], [(xTs[:, k, q * 128:(q + 1) * 128], Wf[:, k, :]) for k in range(16)], T_pb[b], [T_W, T_xTs] + T_Wp)
            s = q % 2
            tile_i = stn * 4 + q
            P.op('dve', (lambda s, b: lambda h: h.tensor_tensor(out=zf[s], in0=pbanks[b][:, 0:8], in1=bf_rep[:], op=ALU.add))(s, b),
                 reads=[T_pb[b], T_const], writes=[T_zf[s]])
            P.op('act', (lambda s: lambda h: h.activation(out=zf[s], in_=zf[s], func=AF.Exp, scale=-1.0))(s), reads=[T_zf[s]], writes=[T_zf[s]])
            P.op('act', (lambda s, tile_i: lambda h: h.activation(out=lf[:, :, tile_i], in_=zf[s], func=AF.Ln, bias=1.0))(s, tile_i),
                 reads=[T_zf[s]], writes=[T_lf])
        dvv = V_s.ap()[:, :, stn * 4:(stn + 1) * 4, :].rearrange("h p q d -> p h q d")
        P.op('sp', (lambda xs, dvv: lambda h: h.dma_start(out=dvv, in_=vbig[xs]))(xs, dvv), reads=[T_vbig[xs]], dkey="vst" + str(xs))
        if DEBUG and stn in (1, 3):
            P.op('sp', (lambda xTs, i: lambda h: h.dma_start(out=XD_s.ap()[i, 1], in_=xTs))(xTs, stn // 2),
                 reads=[T_xTs], dkey="xd1")
    P.barrier()
    if stop == 2:
        return finish()
    AR.off = mark0
    Wrx = AR.alloc([128, 16, 1024], BF16)
    Wa = AR.alloc([128, 8, 128], BF16)
    Wi = AR.alloc([128, 8, 128], BF16)
    dg = AR.alloc([128, 32, 128], BF16)
    T_W = newT("WB")
    T_Wp = []
    load_w(Wrx, COL_RX, 1024, "wrx")
    P.op('pool', lambda h: h.dma_start(out=Wa, in_=w_rec.ap().rearrange("b i j -> i b j")), writes=[T_W], dkey="wa")
    P.op('pool', lambda h: h.dma_start(out=Wi, in_=w_ing.ap().rearrange("b i j -> i b j")), writes=[T_W], dkey="wi")
    for blk in range(8):
        for k in range(4):
            P.op('dve', (lambda blk, k: lambda h: h.tensor_scalar(out=dg[:, blk * 4 + k, :], in0=ident[:], scalar1=convw[:, k, blk:blk + 1],
                                                                 scalar2=None, op0=ALU.mult))(blk, k), reads=[T_const], writes=[T_W])
    xT = [AR.alloc([128, 16, 512], BF16) for _ in range(2)]
    T_xT = [newT("xTB") for _ in range(2)]
    rxh = AR.alloc([128, 8, 516], BF16)
    T_rxh = [newT("rxh") for _ in range(8)]
    rxc = [AR.alloc([128, 512], BF16) for _ in range(2)]
    T_rxc = [newT("rxc") for _ in range(2)]
    rr = [AR.alloc([128, 512], F32) for _ in range(2)]
    T_rr = [newT("rr") for _ in range(2)]
    ig = [AR.alloc([128, 512], F32) for _ in range(2)]
    T_ig = [newT("ig") for _ in range(2)]
    aa = [AR.alloc([128, 512], F32) for _ in range(2)]
    T_aa = [newT("aa") for _ in range(2)]
    sq = [AR.alloc([128, 512], F32) for _ in range(2)]
    T_sq = [newT("sq") for _ in range(2)]
    uu = [AR.alloc([128, 512], F32) for _ in range(2)]
    T_uu = [newT("uu") for _ in range(2)]
    hs = [AR.alloc([128, 512], F32) for _ in range(2)]
    T_hs = [newT("hs") for _ in range(2)]
    hacc = AR.alloc([128, 8, 512], F32)
    T_hacc = [newT("hacc") for _ in range(8)]
    hob = [AR.alloc([128, 512], BF16) for _ in range(2)]
    T_hob = [newT("hob") for _ in range(2)]
    P.op('pool', lambda h: h.memset(rxh, 0.0), writes=T_rxh)
    def rnn_R1(stn, blk):
        xTs, T_xTs = xT[stn % 2], T_xT[stn % 2]
        s = blk % 2
        b = next_bank(0, 6)
        mm_group(pbanks[b][:], [(Wrx[:, k, blk * 128:(blk + 1) * 128], xTs[:, k, :]) for k in range(16)], T_pb[b], [T_W, T_xTs] + T_Wp)
        P.op('act', (lambda blk, b: lambda h: h.activation(out=rxh[:, blk, 3:515], in_=pbanks[b][:], func=AF.Copy))(blk, b),
             reads=[T_pb[b]], writes=[T_rxh[blk]])
        return b

    def rnn_R2(stn, blk):
        s = blk % 2
        b2 = next_bank(0, 6)
        mm_group(pbanks[b2][:], [(dg[:, blk * 4 + k, :], rxh[:, blk, k:k + 512]) for k in range(4)], T_pb[b2], [T_W, T_rxh[blk]])
        P.op('pool', (lambda blk: lambda h: h.tensor_copy(out=rxh[:, blk, 0:3], in_=rxh[:, blk, 512:515]))(blk),
             reads=[T_rxh[blk]], writes=[T_rxh[blk]])
        P.op('act', (lambda s, blk, b2: lambda h: h.activation(out=rxc[s], in_=pbanks[b2][:], func=AF.Identity, bias=convb[:, blk:blk + 1]))(s, blk, b2),
             reads=[T_pb[b2], T_const], writes=[T_rxc[s]])
        b3 = next_bank(0, 6)
        b4 = next_bank(0, 6)
        mm_group(pbanks[b3][:], [(Wa[:, blk, :], rxc[s])], T_pb[b3], [T_W, T_rxc[s]])
        mm_group(pbanks[b4][:], [(Wi[:, blk, :], rxc[s])], T_pb[b4], [T_W, T_rxc[s]])
        P.op('act', (lambda s, blk, b3: lambda h: h.activation(out=rr[s], in_=pbanks[b3][:], func=AF.Sigmoid, bias=ba[:, blk:blk + 1]))(s, blk, b3),
             reads=[T_pb[b3], T_const], writes=[T_rr[s]])
        P.op('act', (lambda s, blk, b4: lambda h: h.activation(out=ig[s], in_=pbanks[b4][:], func=AF.Sigmoid, bias=bi[:, blk:blk + 1]))(s, blk, b4),
             reads=[T_pb[b4], T_const], writes=[T_ig[s]])
        P.op('act', (lambda s, blk: lambda h: h.activation(out=aa[s], in_=rr[s], func=AF.Exp, scale=cch[:, blk:blk + 1]))(s, blk),
             reads=[T_rr[s], T_const], writes=[T_aa[s]])
        P.op('act', (lambda s, blk: lambda h: h.activation(out=sq[s], in_=rr[s], func=AF.Exp, scale=cch2[:, blk:blk + 1]))(s, blk),
             reads=[T_rr[s], T_const], writes=[T_sq[s]])
        P.op('act', (lambda s: lambda h: h.activation(out=sq[s], in_=sq[s], func=AF.Ln, scale=-1.0, bias=1.0))(s),
             reads=[T_sq[s]], writes=[T_sq[s]])
        P.op('act', (lambda s: lambda h: h.activation(out=sq[s], in_=sq[s], func=AF.Exp, scale=0.5))(s),
             reads=[T_sq[s]], writes=[T_sq[s]])
        P.op('pool', (lambda s: lambda h: h.tensor_tensor(out=uu[s], in0=ig[s], in1=rxc[s], op=ALU.mult))(s),
             reads=[T_ig[s], T_rxc[s]], writes=[T_uu[s]])
        P.op('dve', (lambda s: lambda h: h.tensor_tensor(out=uu[s], in0=uu[s], in1=sq[s], op=ALU.mult))(s),
             reads=[T_uu[s], T_sq[s]], writes=[T_uu[s]])
        P.op('dve', (lambda s, blk: lambda h: h.tensor_tensor_scan(out=hs[s], data0=aa[s], data1=uu[s], initial=hstate[:, blk:blk + 1],
                                                                  op0=ALU.mult, op1=ALU.add))(s, blk),
             reads=[T_aa[s], T_uu[s], T_hst], writes=[T_hs[s]])
        P.op('dve', (lambda s, blk: lambda h: h.tensor_copy(out=hstate[:, blk:blk + 1], in_=hs[s][:, 511:512]))(s, blk),
             reads=[T_hs[s]], writes=[T_hst])
        r4 = stn % 4
        if r4 == 0:
            P.op('dve', (lambda s, blk: lambda h: h.tensor_scalar(out=hacc[:, blk, :], in0=hs[s], scalar1=sel[:, 0:1], scalar2=None, op0=ALU.mult))(s, blk),
                 reads=[T_hs[s], T_const], writes=[T_hacc[blk]])
        elif r4 < 3:
            P.op('dve', (lambda s, blk, r4: lambda h: h.scalar_tensor_tensor(out=hacc[:, blk, :], in0=hs[s], scalar=sel[:, r4:r4 + 1], in1=hacc[:, blk, :],
                                                                             op0=ALU.mult, op1=ALU.add))(s, blk, r4),
                 reads=[T_hs[s], T_const, T_hacc[blk]], writes=[T_hacc[blk]])
        else:
            m = stn // 4
            P.op('dve', (lambda s, blk: lambda h: h.scalar_tensor_tensor(out=hob[s], in0=hs[s], scalar=sel[:, 3:4], in1=hacc[:, blk, :],
                                                                         op0=ALU.mult, op1=ALU.add))(s, blk),
                 reads=[T_hs[s], T_const, T_hacc[blk]], writes=[T_hob[s]])
            dv = HOWN_s.ap()[blk, :, m * 512:(m + 1) * 512]
            P.op('sp', (lambda s, dv: lambda h: h.dma_start(out=dv, in_=hob[s]))(s, dv), reads=[T_hob[s]], dkey="hst" + str(s))

    NJ = (S // 512) * 8
    ld('sp', xT[0], XT_s.ap()[0], "xTm0", [T_xT[0]])
    rnn_R1(0, 0)
    for jj in range(NJ):
        if jj % 8 == 0 and jj // 8 + 1 < S // 512:
            sn = jj // 8 + 1
            ld('sp', xT[sn % 2], XT_s.ap()[sn], "xTm" + str(sn % 2), [T_xT[sn % 2]])
        if jj + 1 < NJ:
            rnn_R1((jj + 1) // 8, (jj + 1) % 8)
        rnn_R2(jj // 8, jj % 8)
    P.barrier()
    if stop == 3:
        return finish()

    AR.off = mark0
    Wq = AR.alloc([128, 16, 1024], BF16)
    Wg = AR.alloc([128, 16, 1024], BF16)
    T_W = newT("W4")
    T_Wp = []
    load_w(Wq, 0, 1024, "wq")
    load_w(Wg, COL_RG, 1024, "wg")
    xT = [AR.alloc([128, 16, 512], BF16) for _ in range(2)]
    T_xT = [newT("xT4") for _ in range(2)]
    ksq = [AR.alloc([128, 512], BF16) for _ in range(2)]
    T_ksq = [newT("ksq4") for _ in range(2)]
    kstd = [AR.alloc([128, 512], F32) for _ in range(2)]
    T_kstd = [newT("kstd4") for _ in range(2)]
    ktb = [AR.alloc([128, 512], BF16) for _ in range(2)]
    T_ktb = [newT("ktb4") for _ in range(2)]
    gg = [AR.alloc([128, 512], F32) for _ in range(2)]
    T_gg = [newT("gg") for _ in range(2)]
    hw = [AR.alloc([128, 512], BF16) for _ in range(2)]
    T_hw = [newT("hw") for _ in range(2)]
    ro = [AR.alloc([128, 512], BF16) for _ in range(2)]
    T_ro = [newT("ro") for _ in range(2)]
    for m in range(8):
        xs = m % 2
        xTs, T_xTs = xT[xs], T_xT[xs]
        if m == 0:
            ld('sp', xT[0], XTO_s.ap()[0], "xTo0", [T_xT[0]])
        if m + 1 < 8:
            ld('sp', xT[(m + 1) % 2], XTO_s.ap()[m + 1], "xTo" + str((m + 1) % 2), [T_xT[(m + 1) % 2]])
        for hh in range(8):
            qk_norm_unit(Wq, hh, xTs, T_xTs, qgain[:, 0:1], QT_s.ap()[hh, :, m * 512:(m + 1) * 512], "qst", ucnt[0])
            ucnt[0] += 1
        for blk in range(8):
            s = blk % 2
            b = next_bank(0, 6)
            mm_group(pbanks[b][:], [(Wg[:, k, blk * 128:(blk + 1) * 128], xTs[:, k, :]) for k in range(16)], T_pb[b], [T_W, T_xTs] + T_Wp)
            ld('sp', hw[s], HOWN_s.ap()[blk, :, m * 512:(m + 1) * 512], "hwl" + str(s), [T_hw[s]])
            P.op('act', (lambda s, b: lambda h: h.activation(out=gg[s], in_=pbanks[b][:], func=AF.Gelu_apprx_tanh))(s, b),
                 reads=[T_pb[b]], writes=[T_gg[s]])
            P.op('dve', (lambda s: lambda h: h.tensor_tensor(out=ro[s], in0=gg[s], in1=hw[s], op=ALU.mult))(s),
                 reads=[T_gg[s], T_hw[s]], writes=[T_ro[s]])
            dv = CAT_s.ap()[m // 2, :, 8 + blk, (m % 2) * 512:(m % 2 + 1) * 512]
            P.op('sp', (lambda s, dv: lambda h: h.dma_start(out=dv, in_=ro[s]))(s, dv), reads=[T_ro[s]], dkey="rost" + str(s))

    oi = AR.alloc([128, 128], F32)
    T_oi = newT("oi")
    tmpc = AR.alloc([128, 128], F32)
    T_tmpc = newT("tmpc")
    for hh in range(8):
        bw = next_bank(0, 6)
        bt_ = next_bank(0, 6)
        mm_group(pbanks[bw][:, 0:128], [(tri[:], lf[:, hh, :])], T_pb[bw], [T_const, T_lf])
        mm_group(pbanks[bt_][:, 0:128], [(ones_f[:], lf[:, hh, :])], T_pb[bt_], [T_const, T_lf])
        P.op('dve', (lambda bt_: lambda h: h.tensor_tensor_scan(out=oi, data0=ones_f[:], data1=pbanks[bt_][:, 0:128], initial=0.0,
                                                                op0=ALU.mult, op1=ALU.add))(bt_),
             reads=[T_pb[bt_], T_const], writes=[T_oi])
        P.op('dve', (lambda bw, hh: lambda h: h.tensor_tensor(out=ccol[:, hh, :], in0=pbanks[bw][:, 0:128], in1=oi, op=ALU.add))(bw, hh),
             reads=[T_pb[bw], T_oi], writes=[T_small])
        P.op('dve', (lambda bt_, hh: lambda h: h.tensor_tensor(out=ccol[:, hh, :], in0=ccol[:, hh, :], in1=pbanks[bt_][:, 0:128], op=ALU.subtract))(bt_, hh),
             reads=[T_pb[bt_], T_small], writes=[T_small])
        P.op('dve', lambda h: h.tensor_tensor(out=tmpc, in0=oi, in1=sel16[:], op=ALU.mult), reads=[T_oi, T_const], writes=[T_tmpc])
        P.op('dve', (lambda hh: lambda h: h.tensor_reduce(out=crefp[:, hh, :], in_=tmpc.rearrange("p (m r) -> p m r", r=16), axis=AX.X, op=ALU.add))(hh),
             reads=[T_tmpc], writes=[T_small])
    P.barrier()
    if stop == 4:
        return finish()

    AR.off = mark0
    KT = [AR.alloc([128, S], BF16) for _ in range(2)]
    VV = [AR.alloc([128, 128, 128], BF16) for _ in range(2)]
    maskb = AR.alloc([128, 16, 512], BF16)
    T_mask = newT("mask")
    ld('sp', maskb, c_mask.ap(), "mask", [T_mask])
    QT = [AR.alloc([128, NOWN], BF16) for _ in range(2)]
    T_kvq = [newT("kvq") for _ in range(2)]
    pex = [AR.alloc([128, 512], BF16) for _ in range(4)]
    T_pex = [newT("pex") for _ in range(4)]
    biasb = [AR.alloc([128, 128], F32) for _ in range(2)]
    T_bias = [newT("bias") for _ in range(2)]
    rden = [AR.alloc([128, 512], F32) for _ in range(2)]
    T_rden = [newT("rden") for _ in range(2)]
    ob = [AR.alloc([128, 512], BF16) for _ in range(2)]
    T_ob = [newT("ob") for _ in range(2)]
    pacc = [AR.alloc([128, 512], F32) for _ in range(2)]
    T_pacc = [newT("pacc") for _ in range(2)]
    sidx = [0]
    for hh in range(8):
        hsl = hh % 2
        key = "kvq" + str(hsl)
        for c4 in range(4):
            ld('sp', KT[hsl][:, c4 * 4096:(c4 + 1) * 4096], KT_s.ap()[hh, :, c4 * 4096:(c4 + 1) * 4096], key, [T_kvq[hsl]])
        for c4 in range(4):
            ld('sp', VV[hsl][:, c4 * 32:(c4 + 1) * 32, :], V_s.ap()[hh, :, c4 * 32:(c4 + 1) * 32, :], key, [T_kvq[hsl]])
        ld('sp', QT[hsl], QT_s.ap()[hh, :, :], key, [T_kvq[hsl]])
        for m in range(8):
            nkb = 16 * m + 16
            ms = m % 2
            bO, bD = 4 + 2 * ms, 5 + 2 * ms
            P.op('dve', (lambda ms, hh, m, nkb: lambda h: h.tensor_scalar(out=biasb[ms][:, 0:nkb], in0=ccol[:, hh, 0:nkb], scalar1=crefp[:, hh, m:m + 1],
                                                                          scalar2=None, op0=ALU.subtract))(ms, hh, m, nkb),
                 reads=[T_small], writes=[T_bias[ms]])
            pend = []
            for step in range(nkb + 2):
                if step < nkb:
                    kb = step
                    si = sidx[0] % 4
                    sidx[0] += 1
                    pairs = [(KT[hsl][:, kb * 128:(kb + 1) * 128], QT[hsl][:, m * 512:(m + 1) * 512])]
                    rds = [T_kvq[hsl]]
                    if kb >= 16 * m:
                        pairs.append((ident[:], maskb[:, kb - 16 * m, :]))
                        rds = rds + [T_mask, T_const]
                    mm_group(pbanks[si][:], pairs, T_pb[si], rds)
                    P.op('act', (lambda si, ms, kb: lambda h: h.activation(out=pex[si], in_=pbanks[si][:], func=AF.Exp, bias=biasb[ms][:, kb:kb + 1]))(si, ms, kb),
                         reads=[T_pb[si], T_bias[ms]], writes=[T_pex[si]])
                    pend.append((kb, si))
                if step >= 2:
                    kb, si = pend.pop(0)
                    mm_group(pbanks[bO][:], [(VV[hsl][:, kb, :], pex[si])], T_pb[bO], [T_kvq[hsl], T_pex[si]], first=(kb == 0), last=(kb == nkb - 1))
                    pa_ = kb % 2
                    if kb < 2:
                        P.op('dve', (lambda pa_, si: lambda h: h.tensor_copy(out=pacc[pa_], in_=pex[si]))(pa_, si), reads=[T_pex[si]], writes=[T_pacc[pa_]])
                    else:
                        P.op('dve', (lambda pa_, si: lambda h: h.tensor_tensor(out=pacc[pa_], in0=pacc[pa_], in1=pex[si], op=ALU.add))(pa_, si),
                             reads=[T_pex[si], T_pacc[pa_]], writes=[T_pacc[pa_]])
            mm_group(pbanks[bD][:], [(ones_f[:], pacc[0]), (ones_f[:], pacc[1])], T_pb[bD], [T_const, T_pacc[0], T_pacc[1]])
            P.op('dve', (lambda ms, bD: lambda h: h.reciprocal(out=rden[ms], in_=pbanks[bD][:]))(ms, bD), reads=[T_pb[bD]], writes=[T_rden[ms]])
            P.op('dve', (lambda ms, bO: lambda h: h.tensor_tensor(out=ob[ms], in0=pbanks[bO][:], in1=rden[ms], op=ALU.mult))(ms, bO),
                 reads=[T_pb[bO], T_rden[ms]], writes=[T_ob[ms]])
            dv = CAT_s.ap()[m // 2, :, hh, (m % 2) * 512:(m % 2 + 1) * 512]
            P.op('sp', (lambda ms, dv: lambda h: h.dma_start(out=dv, in_=ob[ms]))(ms, dv), reads=[T_ob[ms]], dkey="obst" + str(ms))
    P.barrier()
    if stop == 5:
        return finish()

    AR.off = mark0
    acc = AR.alloc([128, 8, D], F32)
    T_acc = [newT("acc") for _ in range(8)]
    hnT = AR.alloc([128, 16, 1024], BF16)
    T_hnT = newT("hnT")
    wch = [AR.alloc([128, 16, 512], BF16) for _ in range(2)]
    T_wch = [newT("wch") for _ in range(2)]
    wd = [AR.alloc([128, 2, D], BF16) for _ in range(1)]
    T_wd = [newT("wd") for _ in range(1)]
    grep7 = AR.alloc([128, D], F32)
    T_grep7 = newT("grep7")
    hn = [AR.alloc([128, D], BF16) for _ in range(1)]
    T_hn = [newT("hn") for _ in range(1)]
    wr = AR.alloc([128, 16, 36], BF16)
    T_wr = newT("wr")
    comb = AR.alloc([128, 8, 32], F32)
    T_comb = newT("comb")
    rt = AR.alloc([128, 160], F32)
    T_rt = newT("rt")
    sm7 = [AR.alloc([128, 8], F32) for _ in range(2)]
    T_sm7 = [newT("sm7") for _ in range(2)]
    sgb = [AR.alloc([128, 256], F32) for _ in range(2)]
    T_sgb = [newT("sgb") for _ in range(2)]
    hid = [AR.alloc([128, 256], BF16) for _ in range(2)]
    T_hid = [newT("hid") for _ in range(2)]
    hidT = [AR.alloc([128, 2, 128], BF16) for _ in range(2)]
    T_hidT = [newT("hidT") for _ in range(2)]
    ppf = AR.alloc([128, 256], F32)
    T_ppf = newT("ppf")
    ppb = AR.alloc([128, 256], BF16)
    T_ppb = newT("ppb")
    ppT = AR.alloc([128, 2, 1024], BF16)
    T_ppT = newT("ppT")
    wpu = [AR.alloc([128, 2, 512], BF16) for _ in range(1)]
    T_wpu = [newT("wpu") for _ in range(1)]
    sgm = [AR.alloc([128, 512], F32) for _ in range(1)]
    T_sgm = [newT("sgm") for _ in range(1)]
    catT = hnT
    wst = [AR.alloc([128, 1024], F32) for _ in range(2)]
    wst = wst + [grep7[:, 0:1024], grep7[:, 1024:2048]]
    T_wst = [newT("wst") for _ in range(4)]
    stc = [0]

    def stage_load(src_ap, a, bdim):
        i = stc[0] % 4
        stc[0] += 1
        view = wst[i][:, 0:a * bdim].rearrange("p (a b) -> p a b", a=a)
        ld('sp', view, src_ap, "wst" + str(i), [T_wst[i]])
        return i, view

    def stage_do_cast(eng, dst_ap, view, i, T_dst):
        P.op(eng, lambda h: h.tensor_copy(out=dst_ap, in_=view), reads=[T_wst[i]], writes=[T_dst])

    def stage_cast(dst_ap, src_ap, a, bdim, T_dst, eng='pool'):
        i, view = stage_load(src_ap, a, bdim)
        stage_do_cast(eng, dst_ap, view, i, T_dst)

    for kq in range(4):
        P.op('pool', (lambda kq: lambda h: h.dma_start(out=wr[:, kq * 4:(kq + 1) * 4, 0:4],
                                                       in_=w_rg.ap()[kq * 512:(kq + 1) * 512, :].rearrange("(k p) n -> p k n", p=128)))(kq),
             writes=[T_wr], dkey="wr")
        P.op('pool', (lambda kq: lambda h: h.dma_start(out=wr[:, kq * 4:(kq + 1) * 4, 4:36],
                                                       in_=w_re.ap()[kq * 512:(kq + 1) * 512, :].rearrange("(k p) n -> p k n", p=128)))(kq),
             writes=[T_wr], dkey="wr")

    wrr = [0]

    def load_wchunk(parts):
        s = wrr[0] % 2
        wrr[0] += 1
        for (c0, ncl, src) in parts:
            for kq in range(4):
                for cc in range(0, ncl, 256):
                    srcv = src[kq * 512:(kq + 1) * 512, cc:cc + 256].rearrange("(k p) n -> p k n", p=128)
                    stage_cast(wch[s][:, kq * 4:(kq + 1) * 4, c0 + cc:c0 + cc + 256], srcv, 4, 256, T_wch[s])
        return s

    def norm_block(gain_dram, key):
        ld('sp', grep7, bcast_row(gain_dram, D), key, [T_grep7, T_wst[2], T_wst[3]])
        for t in range(8):
            s = 0
            s_ = sm7[t % 2]
            P.op('act', (lambda t, s_: lambda h: h.activation(out=hn[0], in_=acc[:, t, :], func=AF.Square, accum_out=s_[:, 0:1]))(t, s_),
                 reads=[T_acc[t]], writes=[T_hn[0], T_sm7[t % 2]])
            P.op('dve', (lambda s_: lambda h: h.tensor_scalar(out=s_[:, 1:2], in0=s_[:, 0:1], scalar1=1.0 / D, scalar2=EPS, op0=ALU.mult, op1=ALU.add))(s_),
                 reads=[T_sm7[t % 2]], writes=[T_sm7[t % 2]])
            P.op('act', (lambda s_: lambda h: h.activation(out=s_[:, 2:3], in_=s_[:, 1:2], func=AF.Sqrt))(s_), reads=[T_sm7[t % 2]], writes=[T_sm7[t % 2]])
            P.op('dve', (lambda s_: lambda h: h.reciprocal(out=s_[:, 3:4], in_=s_[:, 2:3]))(s_), reads=[T_sm7[t % 2]], writes=[T_sm7[t % 2]])
            P.op('dve', (lambda t, s, s_: lambda h: h.scalar_tensor_tensor(out=hn[s], in0=acc[:, t, :], scalar=s_[:, 3:4], in1=grep7, op0=ALU.mult, op1=ALU.mult))(t, s, s_),
                 reads=[T_acc[t], T_sm7[t % 2], T_grep7, T_wst[2], T_wst[3]], writes=[T_hn[s]])
            for half in range(2):
                b = next_bank(0, 2)
                transposes(pbanks[b][:].bitcast(BF16), [hn[s][:, (half * 8 + kk) * 128:(half * 8 + kk + 1) * 128] for kk in range(8)], T_pb[b], [T_hn[s]])
                dstv = hnT[:, half * 8:(half + 1) * 8, t * 128:(t + 1) * 128]
                srcv = pbanks[b][:].bitcast(BF16).rearrange("p (a b) -> p a b", a=8)
                P.op('act', (lambda dstv, srcv: lambda h: h.activation(out=dstv, in_=srcv, func=AF.Copy))(dstv, srcv), reads=[T_pb[b]], writes=[T_hnT])


    for tb in range(4):
        o0 = tb * 1024
        ld('sp', catT, CAT_s.ap()[tb], "catl", [T_hnT])
        for t in range(8):
            ld('sp', acc[:, t, :], x_own.ap()[o0 + t * 128:o0 + (t + 1) * 128, :], "accl" + str(t), [T_acc[t]])
        for nch in range(4):
            s = load_wchunk([(0, 512, w_out.ap()[:, nch * 512:(nch + 1) * 512])])
            for t in range(8):
                b = next_bank(0, 2)
                mm_group(pbanks[b][:], [(catT[:, k, t * 128:(t + 1) * 128], wch[s][:, k, :]) for k in range(16)], T_pb[b], [T_hnT, T_wch[s]])
                P.op('dve', (lambda t, nch, b: lambda h: h.tensor_tensor(out=acc[:, t, nch * 512:(nch + 1) * 512], in0=acc[:, t, nch * 512:(nch + 1) * 512],
                                                                        in1=pbanks[b][:], op=ALU.add))(t, nch, b),
                     reads=[T_pb[b], T_acc[t]], writes=[T_acc[t]])
        norm_block(ffn_norm, "gr7")
        for t in range(8):
            b = next_bank(0, 2)
            mm_group(pbanks[b][:, 0:36], [(hnT[:, k, t * 128:(t + 1) * 128], wr[:, k, :]) for k in range(16)], T_pb[b], [T_hnT, T_wr])
            lg = rt[:, 0:36]
            gmax = rt[:, 36:37]
            gsh = rt[:, 40:44]
            gsum = rt[:, 44:45]
            gw = rt[:, 45:46]
            goh = rt[:, 48:52]
            em = rt[:, 56:88]
            m8 = rt[:, 88:96]
            d21 = rt[:, 96:97]
            w1 = rt[:, 97:98]
            w2 = rt[:, 98:99]
            c2 = rt[:, 104:136]
            ge = rt[:, 136:140]

            def R(eng, fn, extra_r=(), extra_w=()):
                P.op(eng, fn, reads=[T_rt] + list(extra_r), writes=[T_rt] + list(extra_w))

            R('dve', (lambda b: lambda h: h.tensor_copy(out=lg, in_=pbanks[b][:, 0:36]))(b), extra_r=[T_pb[b]])
            R('dve', lambda h: h.tensor_reduce(out=gmax, in_=lg[:, 0:4], axis=AX.X, op=ALU.max))
            R('dve', lambda h: h.tensor_scalar(out=gsh, in0=lg[:, 0:4], scalar1=gmax, scalar2=None, op0=ALU.subtract))
            R('act', lambda h: h.activation(out=ge, in_=gsh, func=AF.Exp, accum_out=gsum))
            R('dve', lambda h: h.reciprocal(out=gw, in_=gsum))
            R('dve', lambda h: h.tensor_scalar(out=goh, in0=gsh, scalar1=0.0, scalar2=None, op0=ALU.is_ge))
            R('dve', lambda h: h.tensor_scalar(out=goh, in0=goh, scalar1=-1.0, scalar2=1e9, op0=ALU.add, op1=ALU.mult))
            for g in range(4):
                R('dve', (lambda g: lambda h: h.tensor_scalar(out=em[:, g * 8:(g + 1) * 8], in0=lg[:, 4 + g * 8:4 + (g + 1) * 8], scalar1=goh[:, g:g + 1],
                                                               scalar2=None, op0=ALU.add))(g))
            R('dve', lambda h: h.max(out=m8, in_=em))
            R('dve', lambda h: h.tensor_tensor(out=d21, in0=m8[:, 1:2], in1=m8[:, 0:1], op=ALU.subtract))
            R('act', lambda h: h.activation(out=d21, in_=d21, func=AF.Exp))
            R('dve', lambda h: h.tensor_scalar(out=w1, in0=d21, scalar1=1.0, scalar2=None, op0=ALU.add))
            R('dve', lambda h: h.reciprocal(out=w1, in_=w1))
            R('dve', lambda h: h.tensor_tensor(out=w2, in0=d21, in1=w1, op=ALU.mult))
            R('dve', lambda h: h.tensor_tensor(out=w1, in0=w1, in1=gw, op=ALU.mult))
            R('dve', lambda h: h.tensor_tensor(out=w2, in0=w2, in1=gw, op=ALU.mult))
            R('dve', lambda h: h.tensor_scalar(out=c2, in0=em, scalar1=m8[:, 1:2], scalar2=w2, op0=ALU.is_equal, op1=ALU.mult))
            R('dve', (lambda t: lambda h: h.tensor_scalar(out=comb[:, t, :], in0=em, scalar1=m8[:, 0:1], scalar2=w1, op0=ALU.is_equal, op1=ALU.mult))(t),
              extra_w=[T_comb])
            R('dve', (lambda t: lambda h: h.tensor_tensor(out=comb[:, t, :], in0=comb[:, t, :], in1=c2, op=ALU.add))(t), extra_r=[T_comb], extra_w=[T_comb])
        def wch_pieces(e):
            s_ = e % 2
            out = []
            for (c0, src) in ((0, w_eg.ap()[e]), (256, w_eu.ap()[e])):
                for kq in range(4):
                    out.append((wch[s_][:, kq * 4:(kq + 1) * 4, c0:c0 + 256],
                                src[kq * 512:(kq + 1) * 512, :].rearrange("(k p) n -> p k n", p=128), T_wch[s_]))
            return out

        def wd_pieces(e):
            out = []
            for k2 in range(2):
                for cq in range(2):
                    out.append((wd[0][:, k2:k2 + 1, cq * 1024:(cq + 1) * 1024],
                                w_ed.ap()[e][k2 * 128:(k2 + 1) * 128, cq * 1024:(cq + 1) * 1024].rearrange("(k p) n -> p k n", p=128)))
            return out

        wd_pending = {}

        def stA(i):
            e, t = i // 8, i % 8
            u = i % 2
            b = i % 2
            s_ = e % 2
            mm_group(pbanks[b][:], [(hnT[:, k, t * 128:(t + 1) * 128], wch[s_][:, k, :]) for k in range(16)], T_pb[b], [T_hnT, T_wch[s_]])
            P.op('act', lambda h: h.activation(out=sgb[u], in_=pbanks[b][:, 0:256], func=AF.Silu), reads=[T_pb[b]], writes=[T_sgb[u]])
            P.op('dve', lambda h: h.scalar_tensor_tensor(out=hid[u], in0=pbanks[b][:, 256:512], scalar=comb[:, t, e:e + 1], in1=sgb[u],
                                                         op0=ALU.mult, op1=ALU.mult),
                 reads=[T_pb[b], T_sgb[u], T_comb], writes=[T_hid[u]])

        def stB(i):
            u = i % 2
            bT = 2 + u
            transposes(pbanks[bT][:].bitcast(BF16), [hid[u][:, 0:128], hid[u][:, 128:256]], T_pb[bT], [T_hid[u]])
            P.op('act', lambda h: h.activation(out=hidT[u], in_=pbanks[bT][:].bitcast(BF16)[:, 0:256].rearrange("p (a b) -> p a b", a=2), func=AF.Copy),
                 reads=[T_pb[bT]], writes=[T_hidT[u]])

        def stC(i):
            e, t = i // 8, i % 8
            u = i % 2
            if t == 0:
                if e not in wd_pending:
                    wd_pending[e] = [(dstp2, ) + stage_load(srcp2, 1, 1024) for (dstp2, srcp2) in wd_pieces(e)]
                for (dstp2, si, view) in wd_pending.pop(e):
                    stage_do_cast('dve', dstp2, view, si, T_wd[0])
            for nch in range(4):
                bd = 4 + nch
                mm_group(pbanks[bd][:], [(hidT[u][:, k, :], wd[0][:, k, nch * 512:(nch + 1) * 512]) for k in range(2)], T_pb[bd], [T_hidT[u], T_wd[0]])
                P.op('dve', (lambda nch, bd: lambda h: h.tensor_tensor(out=acc[:, t, nch * 512:(nch + 1) * 512], in0=acc[:, t, nch * 512:(nch + 1) * 512],
                                                                      in1=pbanks[bd][:], op=ALU.add))(nch, bd),
                     reads=[T_pb[bd], T_acc[t]], writes=[T_acc[t]])
            if e + 1 < 32:
                for pi in {0: (0, 1), 1: (2, 3), 2: (4,), 3: (5,), 4: (6,), 5: (7,)}.get(t, ()):
                    dstp, srcp, Tp = wch_pieces(e + 1)[pi]
                    stage_cast(dstp, srcp, 4, 256, Tp, eng='pool')
                if t == 7:
                    wd_pending[e + 1] = [(dstp2, ) + stage_load(srcp2, 1, 1024) for (dstp2, srcp2) in wd_pieces(e + 1)]

        for (dstp, srcp, Tp) in wch_pieces(0):
            stage_cast(dstp, srcp, 4, 256, Tp, eng='pool')
        NST = 256
        stA(0)
        stA(1)
        for i in range(NST):
            stB(i)
            if i + 2 < NST:
                stA(i + 2)
            stC(i)
        norm_block(ple_norm, "gr7")
        for t in range(8):
            ld('sp', ppf, p_own.ap()[o0 + t * 128:o0 + (t + 1) * 128, :], "ppl", [T_ppf])
            P.op('dve', lambda h: h.tensor_copy(out=ppb, in_=ppf), reads=[T_ppf], writes=[T_ppb])
            bT = 2 + t % 2
            transposes(pbanks[bT][:].bitcast(BF16), [ppb[:, 0:128], ppb[:, 128:256]], T_pb[bT], [T_ppb])
            P.op('act', (lambda t, bT: lambda h: h.activation(out=ppT[:, :, t * 128:(t + 1) * 128],
                                                              in_=pbanks[bT][:].bitcast(BF16)[:, 0:256].rearrange("p (a b) -> p a b", a=2), func=AF.Copy))(t, bT),
                 reads=[T_pb[bT]], writes=[T_ppT])
        for nch in range(4):
            s = load_wchunk([(0, 512, w_pg.ap()[:, nch * 512:(nch + 1) * 512])])
            ws = 0
            stage_cast(wpu[ws], w_pu.ap()[:, nch * 512:(nch + 1) * 512].rearrange("(k p) n -> p k n", p=128), 2, 512, T_wpu[ws])
            for t in range(8):
                u = 0
                b = next_bank(0, 2)
                mm_group(pbanks[b][:], [(hnT[:, k, t * 128:(t + 1) * 128], wch[s][:, k, :]) for k in range(16)], T_pb[b], [T_hnT, T_wch[s]])
                b2 = 4 + t % 2
                mm_group(pbanks[b2][:], [(ppT[:, k, t * 128:(t + 1) * 128], wpu[ws][:, k, :]) for k in range(2)], T_pb[b2], [T_ppT, T_wpu[ws]])
                P.op('act', (lambda u, b: lambda h: h.activation(out=sgm[u], in_=pbanks[b][:], func=AF.Sigmoid))(u, b), reads=[T_pb[b]], writes=[T_sgm[u]])
                P.op('dve', (lambda u, b2: lambda h: h.tensor_tensor(out=sgm[u], in0=sgm[u], in1=pbanks[b2][:], op=ALU.mult))(u, b2),
                     reads=[T_pb[b2], T_sgm[u]], writes=[T_sgm[u]])
                P.op('pool', (lambda t, nch, u: lambda h: h.tensor_tensor(out=acc[:, t, nch * 512:(nch + 1) * 512], in0=acc[:, t, nch * 512:(nch + 1) * 512],
                                                                         in1=sgm[u], op=ALU.add))(t, nch, u),
                     reads=[T_sgm[u], T_acc[t]], writes=[T_acc[t]])
        for t in range(8):
            P.op('sp', (lambda t, o0: lambda h: h.dma_start(out=out_own.ap()[o0 + t * 128:o0 + (t + 1) * 128, :], in_=acc[:, t, :]))(t, o0),
                 reads=[T_acc[t]], dkey="outst" + str(t))
    return finish()


_CACHE = {}


def _consts(j):
    selv = np.zeros((128, 4), np.float32)
    selv[:, j] = 1.0
    s16 = np.zeros((128, 8, 16), np.float32)
    s16[:, :, 4 * j + 1] = 1.0
    s16 = s16.reshape(128, 128)
    sl = np.arange(128)[:, None, None]
    r = np.arange(16)[None, :, None]
    t = np.arange(512)[None, None, :]
    mask = np.where(128 * r + sl - t - 512 * j > 0, -30000.0, 0.0).astype(ml_dtypes.bfloat16)
    ident = np.eye(128, dtype=np.float32).astype(ml_dtypes.bfloat16)
    tri = (np.arange(128)[:, None] <= np.arange(128)[None, :]).astype(np.float32)
    return dict(c_sel=selv, c_sel16=s16, c_mask=mask, c_ident=ident, c_tri=tri)


def kernel(**inputs):
    inp = {k: np.asarray(v) for k, v in inputs.items()}
    if "nc" not in _CACHE:
        _CACHE["nc"] = build(int(os.environ.get("K_STOP", "9")))
    nc = _CACHE["nc"]
    x = inp["x"]
    p = inp["p"][0]
    shared = {
        "mix_norm": inp["mix_norm"][0][None, :], "w_in": inp["w_in"][0], "b_forget": inp["b_forget"][0][None, :],
        "q_norm": inp["q_norm"][0][:, None], "k_norm": inp["k_norm"][0][:, None],
        "conv_w": inp["conv_w"][0], "conv_b": inp["conv_b"][0][None, :],
        "w_rec_gate": inp["w_rec_gate"][0], "b_rec_gate": inp["b_rec_gate"][0][None, :],
        "w_in_gate": inp["w_in_gate"][0], "b_in_gate": inp["b_in_gate"][0][None, :],
        "lru_lambda": inp["lru_lambda"][0][None, :], "w_out": inp["w_out"][0], "ffn_norm": inp["ffn_norm"][0][None, :],
        "w_router_group": inp["w_router_group"][0], "w_router_expert": inp["w_router_expert"][0],
        "w_expert_gate": inp["w_expert_gate"][0], "w_expert_up": inp["w_expert_up"][0], "w_expert_down": inp["w_expert_down"][0],
        "ple_norm": inp["ple_norm"][0][None, :], "w_ple_gate": inp["w_ple_gate"][0], "w_ple_up": inp["w_ple_up"][0],
    }
    shared = {k: np.ascontiguousarray(v, dtype=np.float32) for k, v in shared.items()}
    in_maps = []
    for c in range(8):
        b, j = c // 4, c % 4
        xo = np.ascontiguousarray(x[b].reshape(8, 4, 512, D)[:, j].reshape(NOWN, D))
        po = np.ascontiguousarray(p[b].reshape(8, 4, 512, 256)[:, j].reshape(NOWN, 256))
        m = dict(shared)
        m.update(x_all=np.ascontiguousarray(x[b]), x_own=xo, p_own=po)
        m.update(_consts(j))
        in_maps.append(m)
    res = run_bass_kernel_spmd(nc, in_maps, core_ids=list(range(8)))
    out = np.empty((2, S, D), np.float32)
    for c in range(8):
        b, j = c // 4, c % 4
        out[b].reshape(8, 4, 512, D)[:, j] = res.results[c]["out_own"].reshape(8, 512, D)
    if DEBUG:
        _CACHE["res"] = res
    return out
```
